# Optimizing a Trainium2 kernel written in Bass

```python
import jax, jax.numpy as jnp
from jax import lax
import numpy as np

D_MODEL = 1024
BATCH = 8
SEQ = 4096
DEPTH = 2

GRID_W = 64
CTX_LEN = 256
N_MIXERS = 2
HEAD_DIM = 64
N_HEADS = D_MODEL // HEAD_DIM
DECAY_LORA = 64
AAA_LORA = 64
GATE_LORA = 128
CONV_W = 3
N_EXPERTS = 16
N_GROUPS = 4
EXPERTS_PER_GROUP = N_EXPERTS // N_GROUPS
TOP_K = 2
EXPERT_DIM = 512
N_RWKV = (DEPTH + N_MIXERS - 1) // N_MIXERS
N_SCONV = DEPTH // N_MIXERS
RMS_EPS = 1e-6
GN_EPS = 64e-5
L2_EPS = 1e-12

kernel_name = 'hybrid_rwkv7_shortconv_grouped_moe_prefix_dit'


def _rmsnorm(x, g):
    xf = x.astype(jnp.float32)
    xf = xf * lax.rsqrt(jnp.mean(xf * xf, axis=-1, keepdims=True) + RMS_EPS)
    return (xf * g.astype(jnp.float32)).astype(x.dtype)


def _modulate(x, shift, scale):
    return x * (1 + scale) + shift


def _row_neighbours(x, n_rows, row_len):
    b, t, d = x.shape
    xr = x.reshape(b, n_rows, row_len, d)
    pad = jnp.zeros_like(xr[:, :, :1])
    prev = jnp.concatenate([pad, xr[:, :, :-1]], axis=2).reshape(b, t, d)
    nxt = jnp.concatenate([xr[:, :, 1:], pad], axis=2).reshape(b, t, d)
    return prev, nxt


def _heads(z):
    b, t = z.shape[0], z.shape[1]
    return z.astype(jnp.float32).reshape(b, t, N_HEADS, HEAD_DIM)


def _rwkv_shared(xn, n_rows, row_len, mu, w_rkv, g1, g2, k_k):
    prev, nxt = _row_neighbours(xn, n_rows, row_len)
    xx = 0.5 * (prev + nxt) - xn
    x_rkv = jnp.stack([xn + xx * mu[0], xn + xx * mu[1], xn + xx * mu[2]])
    r, k, v = jnp.einsum('pbtd,pde->pbte', x_rkv, w_rkv)
    g = jax.nn.sigmoid((xn + xx * mu[5]) @ g1) @ g2
    r, k, v = _heads(r), _heads(k), _heads(v)
    kk = k * k_k.astype(jnp.float32).reshape(N_HEADS, HEAD_DIM)
    kk = kk * lax.rsqrt(jnp.maximum(jnp.sum(kk * kk, axis=-1, keepdims=True), L2_EPS * L2_EPS))
    return xx, r, k, v, kk, g


def _rwkv_direction(xn, xx, mu, k, w0, w1, w2, a0, a1, a2, k_a):
    xw = xn + xx * mu[3]
    xa = xn + xx * mu[4]
    w_log = -jax.nn.softplus(-(w0 + jnp.tanh(xw @ w1) @ w2).astype(jnp.float32)) - 0.5
    decay = _heads(jnp.exp(-jnp.exp(w_log)))
    a = _heads(jax.nn.sigmoid((a0 + (xa @ a1) @ a2).astype(jnp.float32)))
    k_dir = k * (1 + (a - 1) * k_a.astype(jnp.float32).reshape(N_HEADS, HEAD_DIM))
    return decay, a, k_dir


def _wkv_scan(decay, k, v, kk, a, r, s0, reverse):
    with_output = r is not None
    xs = jax.tree_util.tree_map(lambda z: jnp.moveaxis(z, 1, 0), (decay, k, v, kk, a, r))

    def step(s, inp):
        w_t, k_t, v_t, kk_t, a_t, r_t = inp
        sa = jnp.einsum('bhvk,bhk->bhv', s, -kk_t)
        s = (s * w_t[:, :, None, :] + sa[..., None] * (kk_t * a_t)[:, :, None, :]
             + v_t[..., None] * k_t[:, :, None, :])
        y = jnp.einsum('bhvk,bhk->bhv', s, r_t) if with_output else None
        return s, y

    s_final, ys = lax.scan(step, s0, xs, reverse=reverse)
    return s_final, (jnp.moveaxis(ys, 0, 1) if with_output else None)


def _bonus(r, k_dir, v, r_k):
    return jnp.sum(r * k_dir * r_k.astype(jnp.float32), axis=-1, keepdims=True) * v


def _rwkv_readout(y, g, gn_w, gn_b, w_o, dtype):
    b, t = y.shape[0], y.shape[1]
    mean = jnp.mean(y, axis=-1, keepdims=True)
    var = jnp.mean(jnp.square(y - mean), axis=-1, keepdims=True)
    y = ((y - mean) * lax.rsqrt(var + GN_EPS)).reshape(b, t, D_MODEL)
    y = (y * gn_w.astype(jnp.float32) + gn_b.astype(jnp.float32)).astype(dtype)
    return (y * g) @ w_o


def _rwkv_mixer(xn, n_rows, xnc, ctx_out, mu, w_rkv, w0, w1, w2, a0, a1, a2, g1, g2,
                k_k, k_a, r_k, gn_w, gn_b, w_o):
    ctx_len = xnc.shape[1]
    xx_l, r_l, k_l, v_l, kk_l, g_l = _rwkv_shared(xn, n_rows, GRID_W, mu, w_rkv, g1, g2, k_k)
    xx_c, r_c, k_c, v_c, kk_c, g_c = _rwkv_shared(xnc, 1, ctx_len, mu, w_rkv, g1, g2, k_k)
    s0 = jnp.zeros((xn.shape[0], N_HEADS, HEAD_DIM, HEAD_DIM), jnp.float32)
    y_lat, y_ctx = [], []
    for p, reverse in enumerate((False, True)):
        dec_l, a_l, kd_l = _rwkv_direction(xn, xx_l, mu, k_l, w0[p], w1[p], w2[p], a0[p], a1[p], a2[p], k_a)
        dec_c, a_c, kd_c = _rwkv_direction(xnc, xx_c, mu, k_c, w0[p], w1[p], w2[p], a0[p], a1[p], a2[p], k_a)
        s_ctx, yc = _wkv_scan(dec_c, kd_c, v_c, kk_c, a_c, r_c if ctx_out else None, s0, reverse)
        _, yl = _wkv_scan(dec_l, kd_l, v_l, kk_l, a_l, r_l, s_ctx, reverse)
        y_lat.append(yl + _bonus(r_l, kd_l, v_l, r_k))
        if ctx_out:
            y_ctx.append(yc + _bonus(r_c, kd_c, v_c, r_k))
    out_lat = _rwkv_readout(y_lat[0] + y_lat[1], g_l, gn_w, gn_b, w_o, xn.dtype)
    out_ctx = _rwkv_readout(y_ctx[0] + y_ctx[1], g_c, gn_w, gn_b, w_o, xnc.dtype) if ctx_out else None
    return out_lat, out_ctx


def _short_conv(xn, n_rows, row_len, w_in, conv_w, w_out):
    bg, cg, xin = jnp.split(xn @ w_in, 3, axis=-1)
    u = cg * xin
    prev, nxt = _row_neighbours(u, n_rows, row_len)
    conv = conv_w[0] * prev + conv_w[1] * u + conv_w[2] * nxt
    return (bg * conv) @ w_out


def _moe(h, router_w, router_b, w_gate, w_up, w_down):
    b, t, d = h.shape
    hf = h.reshape(b * t, d)
    s = jax.nn.sigmoid(jnp.einsum('nd,de->ne', hf.astype(jnp.float32), router_w.astype(jnp.float32)))
    sel = (s + router_b.astype(jnp.float32)).reshape(-1, N_GROUPS, EXPERTS_PER_GROUP)
    group_score = jnp.sum(lax.top_k(sel, TOP_K)[0], axis=-1)
    g_idx = jnp.argmax(group_score, axis=-1)
    in_group = jnp.take_along_axis(sel, g_idx[:, None, None], axis=1)[:, 0]
    _, local = lax.top_k(in_group, TOP_K)
    experts = g_idx[:, None] * EXPERTS_PER_GROUP + local
    gate = jnp.take_along_axis(s, experts, axis=1)
    gate = gate / jnp.sum(gate, axis=-1, keepdims=True)
    combine = jnp.einsum('nk,nke->ne', gate, jax.nn.one_hot(experts, N_EXPERTS, dtype=jnp.float32)).astype(h.dtype)
    out = jnp.zeros_like(hf)
    for e in range(N_EXPERTS):
        he = jax.nn.silu(hf @ w_gate[e]) * (hf @ w_up[e])
        out = out + combine[:, e:e + 1] * (he @ w_down[e])
    return out.reshape(b, t, d)


def setup_inputs(seed: int = 0) -> dict:
    key = jax.random.key(seed)
    ks = iter(jax.random.split(key, 48))

    def nrm(shape, scale):
        return scale * jax.random.normal(next(ks), shape, jnp.float32)

    def uni(shape, lo, hi):
        return jax.random.uniform(next(ks), shape, jnp.float32, lo, hi)

    d, na, nb = D_MODEL, N_RWKV, N_SCONV
    inv = d ** -0.5
    return {
        'x': nrm((BATCH, SEQ, d), 1.0),
        'c': nrm((BATCH, d), 1.0),
        'ctx': nrm((BATCH, CTX_LEN, d), 1.0),
        'c_ctx': nrm((d,), 1.0),
        'ada_w': nrm((DEPTH, d, 6 * d), 0.5 * inv),
        'ada_b': nrm((DEPTH, 6 * d), 0.01),
        'norm_g': 1.0 + nrm((DEPTH, 2, d), 0.02),
        'rw_mu': uni((na, 6, d), 0.0, 1.0),
        'rw_w_rkv': nrm((na, 3, d, d), inv),
        'rw_w0': uni((na, 2, d), -6.0, -1.0),
        'rw_w1': nrm((na, 2, d, DECAY_LORA), inv),
        'rw_w2': nrm((na, 2, DECAY_LORA, d), 0.1 * DECAY_LORA ** -0.5),
        'rw_a0': nrm((na, 2, d), 0.1),
        'rw_a1': nrm((na, 2, d, AAA_LORA), inv),
        'rw_a2': nrm((na, 2, AAA_LORA, d), 0.5 * AAA_LORA ** -0.5),
        'rw_g1': nrm((na, d, GATE_LORA), inv),
        'rw_g2': nrm((na, GATE_LORA, d), GATE_LORA ** -0.5),
        'rw_k_k': 0.85 + nrm((na, d), 0.05),
        'rw_k_a': 1.0 + nrm((na, d), 0.05),
        'rw_r_k': nrm((na, N_HEADS, HEAD_DIM), 0.1),
        'rw_gn_w': 1.0 + nrm((na, d), 0.02),
        'rw_gn_b': nrm((na, d), 0.01),
        'rw_w_o': nrm((na, d, d), inv),
        'sc_w_in': nrm((nb, d, 3 * d), inv),
        'sc_conv': nrm((nb, CONV_W, d), CONV_W ** -0.5),
        'sc_w_out': nrm((nb, d, d), inv),
        'router_w': nrm((d, N_EXPERTS), inv),
        'router_b': nrm((N_EXPERTS,), 0.01),
        'moe_w_gate': nrm((DEPTH, N_EXPERTS, d, EXPERT_DIM), inv),
        'moe_w_up': nrm((DEPTH, N_EXPERTS, d, EXPERT_DIM), inv),
        'moe_w_down': nrm((DEPTH, N_EXPERTS, EXPERT_DIM, d), EXPERT_DIM ** -0.5),
        'final_g': 1.0 + nrm((d,), 0.02),
    }


def reference(x, c, ctx, c_ctx, ada_w, ada_b, norm_g, rw_mu, rw_w_rkv, rw_w0, rw_w1, rw_w2,
              rw_a0, rw_a1, rw_a2, rw_g1, rw_g2, rw_k_k, rw_k_a, rw_r_k, rw_gn_w, rw_gn_b, rw_w_o,
              sc_w_in, sc_conv, sc_w_out, router_w, router_b, moe_w_gate, moe_w_up, moe_w_down,
              final_g):
    rows = x.shape[1] // GRID_W
    ctx_len = ctx.shape[1]
    mod_lat = jnp.einsum('bd,lde->lbe', jax.nn.silu(c), ada_w) + ada_b[:, None, :]
    mod_ctx = jnp.einsum('d,lde->le', jax.nn.silu(c_ctx), ada_w) + ada_b
    h, hc = x, ctx
    for i in range(DEPTH):
        kind = i % N_MIXERS
        idx = i // N_MIXERS
        ctx_live = any(j % N_MIXERS == 0 for j in range(i + 1, DEPTH))
        sh1, sc1, gt1, sh2, sc2, gt2 = jnp.split(mod_lat[i][:, None, :], 6, axis=-1)
        csh1, csc1, cgt1, csh2, csc2, cgt2 = jnp.split(mod_ctx[i][None, None, :], 6, axis=-1)
        xn = _modulate(_rmsnorm(h, norm_g[i, 0]), sh1, sc1)
        xnc = _modulate(_rmsnorm(hc, norm_g[i, 0]), csh1, csc1) if (kind == 0 or ctx_live) else None
        if kind == 0:
            y, yc = _rwkv_mixer(xn, rows, xnc, ctx_live, rw_mu[idx], rw_w_rkv[idx], rw_w0[idx],
                                rw_w1[idx], rw_w2[idx], rw_a0[idx], rw_a1[idx], rw_a2[idx],
                                rw_g1[idx], rw_g2[idx], rw_k_k[idx], rw_k_a[idx], rw_r_k[idx],
                                rw_gn_w[idx], rw_gn_b[idx], rw_w_o[idx])
        else:
            y = _short_conv(xn, rows, GRID_W, sc_w_in[idx], sc_conv[idx], sc_w_out[idx])
            yc = _short_conv(xnc, 1, ctx_len, sc_w_in[idx], sc_conv[idx], sc_w_out[idx]) if ctx_live else None
        h = h + gt1 * y
        h = h + gt2 * _moe(_modulate(_rmsnorm(h, norm_g[i, 1]), sh2, sc2), router_w, router_b,
                           moe_w_gate[i], moe_w_up[i], moe_w_down[i])
        if ctx_live:
            hc = hc + cgt1 * yc
            hc = hc + cgt2 * _moe(_modulate(_rmsnorm(hc, norm_g[i, 1]), csh2, csc2), router_w, router_b,
                                  moe_w_gate[i], moe_w_up[i], moe_w_down[i])
    return _rmsnorm(h, final_g)
```

```python
import contextlib
import numpy as np
import concourse.bass as bass
import concourse.mybir as mybir
from concourse.bass_utils import run_bass_kernel_spmd

F32 = mybir.dt.float32
BF16 = mybir.dt.bfloat16
ALU = mybir.AluOpType
AF = mybir.ActivationFunctionType
AX = mybir.AxisListType

NCORE = 8
T = 4096
D = 1024
FC = 8
CTXL = 256
NCH = 34
NE = 16
EPS = 1e-6
GN_EPS = 64e-5
DECAY_C = float(np.exp(-0.5))
NV = 23


class Buf:
    __slots__ = ("w", "r", "ds", "dram", "excl")

    def __init__(self, dram=False, excl=False):
        self.w = {}
        self.r = {}
        self.ds = None
        self.dram = dram
        self.excl = excl


def PBuf():
    return Buf(excl=True)


class _Unused:
    pass


class DSem:
    __slots__ = ("sem", "cnt")

    def __init__(self, sem):
        self.sem = sem
        self.cnt = 0


class Sched:
    ENG = ("pe", "dve", "act", "pool", "sp")

    def __init__(self, nc, es, n_dma_sems=88):
        self.nc = nc
        self.eng = {"pe": nc.tensor, "dve": nc.vector, "act": nc.scalar, "pool": nc.gpsimd, "sp": nc.sync}
        self.sem = {k: es.enter_context(nc.semaphore("s_" + k)) for k in self.ENG}
        self.cnt = {k: 0 for k in self.ENG}
        self.waited = {k: {} for k in self.ENG}
        self.engsem = set(id(s) for s in self.sem.values())
        self.free_ds = [DSem(es.enter_context(nc.semaphore("d%d" % i))) for i in range(n_dma_sems)]
        self.all_ds = list(self.free_ds)
        self.n_ops = 0
        self.bar_done = {}
        self.free_q = {k: [] for k in self.ENG}
        self.ds_bufs = []

    def get_ds(self, q):
        if self.free_q[q]:
            return self.free_q[q].pop()
        return self.free_ds.pop()

    def release(self, bufs):
        for b in bufs:
            if b.ds is not None:
                for q, d in b.ds.items():
                    self.free_q[q].append(d)
                b.ds = None

    def _waits(self, e, reads, writes, partial=False):
        deps = {}
        for b in reads:
            for s, v in b.w.items():
                if deps.get(s, (None, 0))[1] < v:
                    deps[s] = (s, v)
        for b in writes:
            if not partial:
                for s, v in b.w.items():
                    if deps.get(s, (None, 0))[1] < v:
                        deps[s] = (s, v)
            for s, v in b.r.items():
                if deps.get(s, (None, 0))[1] < v:
                    deps[s] = (s, v)
        wd = self.waited[e]
        out = []
        for s, v in deps.values():
            if wd.get(s, 0) >= v or self.bar_done.get(s, 0) >= v:
                continue
            wd[s] = v
            out.append((s, v))
        return out

    def op(self, e, fn, reads=(), writes=(), pe_accum=False):
        ex = [b for b in reads if b.excl]
        if ex:
            writes = list(writes) + ex
            reads = [b for b in reads if not b.excl]
        waits = self._waits(e, reads, writes)
        eng = self.eng[e]
        for s, v in waits:
            if pe_accum and s is self.sem[e]:
                continue
            eng.wait_ge(s, v)
        ins = fn(eng)
        self.cnt[e] += 1
        s = self.sem[e]
        c = self.cnt[e]
        ins.then_inc(s, 1)
        for b in reads:
            if b.r.get(s, 0) < c:
                b.r[s] = c
        for b in writes:
            b.w = {s: c}
            b.r = {}
        self.n_ops += 1

    def dma(self, q, out, in_, reads=(), writes=(), sembuf=None):
        waits = self._waits(q, reads, writes, partial=True)
        eng = self.eng[q]
        for s, v in waits:
            eng.wait_ge(s, v)
        sb = sembuf
        if sb is None:
            cands = [b for b in list(writes) + list(reads) if not b.dram]
            sb = cands[0] if cands else (writes[0] if writes else reads[0])
        if sb.ds is None:
            sb.ds = {}
        if q not in sb.ds:
            sb.ds[q] = self.get_ds(q)
            if sb not in self.ds_bufs:
                self.ds_bufs.append(sb)
        dsq = sb.ds[q]
        dsq.cnt += 16
        s, c = dsq.sem, dsq.cnt
        eng.dma_start(out=out, in_=in_).then_inc(s, 16)
        for b in reads:
            if b.r.get(s, 0) < c:
                b.r[s] = c
        for b in writes:
            if b.w.get(s, 0) < c:
                b.w[s] = c
        self.n_ops += 1

    def barrier(self):
        targets = [(self.sem[k], self.cnt[k]) for k in self.ENG if self.cnt[k] > 0]
        targets += [(d.sem, d.cnt) for d in self.all_ds if d.cnt > 0]
        for e in self.ENG:
            wd = self.waited[e]
            for s, v in targets:
                if s is self.sem[e]:
                    continue
                if wd.get(s, 0) >= v:
                    continue
                wd[s] = v
                self.eng[e].wait_ge(s, v)
        for s, v in targets:
            self.bar_done[s] = v
        for b in self.ds_bufs:
            if b.ds is not None:
                for q, d in b.ds.items():
                    self.free_q[q].append(d)
                b.ds = None
        self.ds_bufs = []

    def final_wait(self, e, bufs):
        for s, v in self._waits(e, bufs, ()):
            self.eng[e].wait_ge(s, v)


def lockstep_gen(gens):
    gens = list(gens)
    while gens:
        for g in list(gens):
            try:
                next(g)
            except StopIteration:
                gens.remove(g)
        yield


def run_lockstep(gens):
    gens = list(gens)
    while gens:
        for g in list(gens):
            try:
                next(g)
            except StopIteration:
                gens.remove(g)


class Ring:
    def __init__(self, items):
        self.items = items
        self.i = 0

    def next(self):
        it = self.items[self.i % len(self.items)]
        self.i += 1
        return it


def build_program(dbg=None, stop_after=None):
    nc = bass.Bass("TRN2", target_bir_lowering=False)

    def din(name, shape, dt=F32):
        return nc.dram_tensor(name, list(shape), dt, kind="ExternalInput").ap()

    def dscr(name, shape, dt=F32):
        return nc.dram_tensor(name, list(shape), dt, kind="Internal").ap()

    x_d = din("x", [T, D])
    ctx_d = din("ctx", [CTXL, D])
    cvec_d = din("cvec", [128, 16])
    adaw_d = din("ada_w", [2, D, 6 * D])
    adab_d = din("adab", [128, 96])
    vecs_d = din("vecs", [128, NV * 8])
    cst_d = din("cst", [128, 1152])
    rtb_d = din("rtb", [128, 16])
    wrkv_d = din("w_rkv", [3, D, D])
    w1_d = din("w1", [2, D, 64])
    w2_d = din("w2", [2, 64, D])
    a1_d = din("a1", [2, D, 64])
    a2_d = din("a2", [2, 64, D])
    g1_d = din("g1", [D, 128])
    g2_d = din("g2", [128, D])
    wo_d = din("w_o", [D, D])
    win_d = din("w_in", [D, 3 * D])
    wout_d = din("w_out", [D, D])
    rw_d = din("router_w", [D, NE])
    mg_d = din("moe_g", [2, NE, D, 512])
    mu_d = din("moe_u", [2, NE, D, 512])
    md_d = din("moe_d", [2, NE, 512, D])
    out_d = nc.dram_tensor("out", [T, D], F32, kind="ExternalOutput").ap()
    dbg_d = {}
    if dbg:
        for name, shape in dbg.items():
            dbg_d[name] = nc.dram_tensor(name, list(shape), F32, kind="ExternalOutput").ap()

    hT_d = dscr("hT_s", [128, FC, T])
    blk_d = dscr("blk_s", [2, NCH, 128, FC * 6 * 128], BF16)
    vtm_d = dscr("vtm_s", [NCH, 128, D], BF16)
    gT_d = dscr("gT_s", [128, FC, T], BF16)
    bon_d = dscr("bon_s", [128, FC, T])
    yT_d = dscr("yT_s", [2, 128, FC, T])

    B_hT, B_blk, B_vtm, B_gT, B_bon, B_yT, B_out = [Buf(dram=True) for _ in range(7)]
    B_dbg = {k: Buf(dram=True) for k in dbg_d}

    es = contextlib.ExitStack()
    with es:
        S = Sched(nc, es)

        uid = {"n": 0}

        def sbt(stack, name, shape, dt):
            uid["n"] += 1
            return stack.enter_context(nc.sbuf_tensor("t%d_%s" % (uid["n"], name), list(shape), dt))

        def pst(stack, name, shape, dt):
            uid["n"] += 1
            return stack.enter_context(nc.psum_tensor("p%d_%s" % (uid["n"], name), list(shape), dt))

        def pslots2(stack, name, n, width=256):
            out = []
            per = 512 // width
            for i in range((n + per - 1) // per):
                t_ = pst(stack, "%s%d" % (name, i), [128, per, width], F32)
                bb_ = PBuf()
                for j in range(per):
                    if len(out) < n:
                        out.append((t_[:, j, :], bb_))
            return Ring(out)

        dmaq = Ring(["sp"])

        cst = sbt(es, "cst", [128, 1152], F32)
        B_cst = Buf()
        cstb = sbt(es, "cstb", [128, 384], BF16)
        B_cstb = Buf()
        vecs = sbt(es, "vecs", [128, NV, 8], F32)
        B_vecs = Buf()
        mod = sbt(es, "mod", [128, 2, 48, 2], F32)
        B_mod = Buf()
        der = sbt(es, "der", [128, 5, 8], F32)
        B_der = Buf()
        rtb = sbt(es, "rtb", [128, NE], F32)
        B_rtb = Buf()
        rwt = sbt(es, "rwt", [128, FC, NE], F32)
        B_rwt = Buf()

        S.dma("sp", cst[:], cst_d, writes=[B_cst])
        S.dma("sp", vecs[:].rearrange("p v c -> p (v c)"), vecs_d, writes=[B_vecs])
        S.dma("sp", rtb[:], rtb_d, writes=[B_rtb])
        S.dma("sp", rwt[:], rw_d.rearrange("(c p) e -> p c e", p=128), writes=[B_rwt])
        identf = cst[:, 0:128]
        ident_b = cstb[:, 0:128]
        blk64_b = cstb[:, 128:256]
        ones_b = cstb[:, 256:384]
        S.op("dve", lambda e: e.tensor_copy(out=cstb[:, 0:128], in_=cst[:, 0:128]), [B_cst], [B_cstb])
        S.op("dve", lambda e: e.tensor_copy(out=cstb[:, 128:384], in_=cst[:, 640:896]), [B_cst], [B_cstb])
        MB = [cst[:, 128:384], cst[:, 384:640]]
        MT = [cst[:, 384:512], cst[:, 128:256]]
        scanmask = cst[:, 896:1152]

        def vec(i):
            return vecs[:, i, :]

        epsc = sbt(es, "epsc", [128, 2], F32)
        B_epsc = Buf()
        S.op("pool", lambda e: e.memset(epsc[:, 0:1], EPS), [], [B_epsc])
        S.op("pool", lambda e: e.memset(epsc[:, 1:2], GN_EPS), [], [B_epsc])
        omk = sbt(es, "omk", [128, 8], F32)
        B_omk = Buf()
        S.op("dve", lambda e: e.tensor_scalar(out=omk[:], in0=vecs[:, 15, :], scalar1=-1.0, scalar2=1.0, op0=ALU.mult, op1=ALU.add), [B_vecs], [B_omk])

        rr = {"i": 0}

        def ew_eng(psum=False, allow=("dve", "act", "pool")):
            cands = [a for a in allow if not (psum and a == "pool")]
            rr["i"] += 1
            return cands[rr["i"] % len(cands)]

        def copy(e, out, in_, r, w):
            if e == "act":
                S.op("act", lambda g: g.activation(out=out, in_=in_, func=AF.Copy), r, w)
            else:
                S.op(e, lambda g: g.tensor_copy(out=out, in_=in_), r, w)

        def load_w(stack_stage, dst, dst_buf, src_ap, shape, name):
            st, bst = stack_stage.next()
            n = int(np.prod(shape[1:]))
            stv = st[:, 0:n]
            if len(shape) == 3:
                stv = stv.rearrange("p (a b) -> p a b", a=shape[1])
            S.dma(dmaq.next(), stv[0:shape[0]], src_ap, writes=[bst])
            copy(ew_eng(False, ("pool", "dve", "act")), dst, stv[0:shape[0]], [bst], [dst_buf])

        with contextlib.ExitStack() as ph:
            cv = sbt(ph, "cv", [128, 8, 2], F32)
            B_cv = Buf()
            sv = sbt(ph, "sv", [128, 8, 2], F32)
            B_sv = Buf()
            adab = sbt(ph, "adab", [128, 2, 48], F32)
            B_adab = Buf()
            S.dma("sp", cv[:].rearrange("p a b -> p (a b)"), cvec_d, writes=[B_cv])
            S.dma("sp", adab[:].rearrange("p a b -> p (a b)"), adab_d, writes=[B_adab])
            S.op("act", lambda e: e.activation(out=sv[:], in_=cv[:], func=AF.Silu), [B_cv], [B_sv])
            slabs = Ring([(sbt(ph, "slab%d" % i, [128, 8, 1024], F32), Buf()) for i in range(2)])
            pm = pst(ph, "pm", [128, 8, 2], F32)
            B_pm = PBuf()
            for l in range(2):
                for sl in range(6):
                    slab, bsl = slabs.next()
                    src = adaw_d[l, :, sl * 1024:(sl + 1) * 1024].rearrange("(c p) e -> p c e", p=128)
                    S.dma("sp", slab[:, 0:4, :], src[:, 0:4, :], writes=[bsl])
                    S.dma("sp", slab[:, 4:8, :], src[:, 4:8, :], writes=[bsl])
                    for ec in range(8):
                        for dc in range(8):
                            S.op("pe", lambda e, ec=ec, dc=dc, slab=slab: e.matmul(
                                pm[:, ec, :], lhsT=slab[:, dc, ec * 128:(ec + 1) * 128], rhs=sv[:, dc, :],
                                start=(dc == 0), stop=(dc == 7)),
                                [bsl, B_sv], [B_pm], pe_accum=True)
                    for j in range(2):
                        S.op("dve", lambda e, l=l, sl=sl, j=j: e.tensor_tensor(
                            out=mod[:, l, sl * 8:(sl + 1) * 8, j], in0=pm[:, :, j],
                            in1=adab[:, l, sl * 8:(sl + 1) * 8], op=ALU.add),
                            [B_pm, B_adab], [B_mod])
            for l in range(2):
                for s_ in range(2):
                    S.op("dve", lambda e, l=l, s_=s_: e.scalar_tensor_tensor(
                        out=der[:, l * 2 + s_, :], in0=mod[:, l, (1 + 3 * s_) * 8:(2 + 3 * s_) * 8, 0], scalar=1.0,
                        in1=vec(l * 2 + s_), op0=ALU.add, op1=ALU.mult), [B_mod, B_vecs], [B_der])
            S.op("dve", lambda e: e.scalar_tensor_tensor(
                out=der[:, 4, :], in0=mod[:, 0, 8:16, 1], scalar=1.0, in1=vec(0), op0=ALU.add, op1=ALU.mult),
                [B_mod, B_vecs], [B_der])
            if dbg and "d_mod" in dbg_d:
                S.dma("sp", dbg_d["d_mod"], mod[:].rearrange("p a b c -> p (a b c)"), reads=[B_mod], writes=[B_dbg["d_mod"]])
            S.barrier()

        def finish(bufs):
            if "d_hT" in dbg_d:
                S.dma("sp", dbg_d["d_hT"], hT_d.rearrange("p c t -> p (c t)"), reads=[B_hT], writes=[B_dbg["d_hT"]])
            S.final_wait("sp", list(bufs) + list(B_dbg.values()))
            return nc

        if stop_after == "prologue":
            return finish([])

        def sc_ap(l, s_, fc):
            return der[:, l * 2 + s_, fc:fc + 1]

        def sh_ap(l, s_, fc, j=0):
            return mod[:, l, 3 * s_ * 8 + fc, j:j + 1]

        def gt_ap(l, s_, fc):
            return mod[:, l, (2 + 3 * s_) * 8 + fc, 0:1]

        def norm_mod(hx, b_hx, nt, xn, b_xn, scale_fn, shift_fn, sqt, b_sq, rstd, b_rstd, pn, b_pn):
            S.op("act", lambda e: e.activation(out=sqt[:, :, 0:nt], in_=hx[:, :, 0:nt], func=AF.Square),
                 [b_hx], [b_sq])
            for fc in range(8):
                S.op("pe", lambda e, fc=fc: e.matmul(pn[:, 0:nt], lhsT=ones_b, rhs=sqt[:, fc, 0:nt],
                                                      start=(fc == 0), stop=(fc == 7)),
                     [b_sq, B_cstb], [b_pn], pe_accum=True)
            S.op("act", lambda e: e.activation(out=rstd[:, 0:nt], in_=pn[:, 0:nt], func=AF.Sqrt, bias=epsc[:, 0:1], scale=1.0 / D),
                 [b_pn, B_epsc], [b_rstd])
            S.op("dve", lambda e: e.reciprocal(out=rstd[:, 0:nt], in_=rstd[:, 0:nt]), [b_rstd], [b_rstd])
            for fc in range(8):
                if fc % 2 == 0:
                    S.op("dve", lambda e, fc=fc: e.scalar_tensor_tensor(
                        out=xn[:, fc, 0:nt], in0=hx[:, fc, 0:nt], scalar=scale_fn(fc), in1=rstd[:, 0:nt],
                        op0=ALU.mult, op1=ALU.mult), [b_hx, b_rstd, B_der, B_mod, B_vecs], [b_xn])
                else:
                    S.op("act", lambda e, fc=fc: e.activation(
                        out=xn[:, fc, 0:nt], in_=hx[:, fc, 0:nt], func=AF.Copy, scale=scale_fn(fc)),
                        [b_hx, B_der, B_mod, B_vecs], [b_xn])
                    S.op("pool", lambda e, fc=fc: e.tensor_tensor(
                        out=xn[:, fc, 0:nt], in0=xn[:, fc, 0:nt], in1=rstd[:, 0:nt], op=ALU.mult), [b_xn, b_rstd], [b_xn])
                if shift_fn is not None:
                    S.op("act", lambda e, fc=fc: e.activation(out=xn[:, fc, 0:nt], in_=xn[:, fc, 0:nt],
                                                                func=AF.Identity, bias=shift_fn(fc), scale=1.0),
                         [b_xn, B_mod], [b_xn])

        NT = 256
        gam = sbt(es, "gam", [128, 2, NCH, 8], F32)
        B_gam = Buf()
        with contextlib.ExitStack() as ph:
            wrkv = sbt(ph, "wrkv", [128, 3, 8, 1024], BF16)
            w1 = sbt(ph, "w1", [128, 2, 8, 64], BF16)
            w2 = sbt(ph, "w2", [64, 2, 1024], BF16)
            a1 = sbt(ph, "a1", [128, 2, 8, 64], BF16)
            a2 = sbt(ph, "a2", [64, 2, 1024], BF16)
            g1 = sbt(ph, "g1", [128, 8, 128], BF16)
            g2 = sbt(ph, "g2", [128, 1024], BF16)
            B_w = Buf()
            with contextlib.ExitStack() as ph2:
                stg = Ring([(sbt(ph2, "stg%d" % i, [128, 8192], F32), Buf()) for i in range(2)])
                for p_ in range(3):
                    load_w(stg, wrkv[:, p_, :, :], B_w, wrkv_d[p_].rearrange("(c p) e -> p c e", p=128), [128, 8, 1024], "wrkv")
                for d_ in range(2):
                    load_w(stg, w1[:, d_, :, :], B_w, w1_d[d_].rearrange("(c p) e -> p c e", p=128), [128, 8, 64], "w1")
                    load_w(stg, a1[:, d_, :, :], B_w, a1_d[d_].rearrange("(c p) e -> p c e", p=128), [128, 8, 64], "a1")
                    load_w(stg, w2[:, d_, :], B_w, w2_d[d_], [64, 1024], "w2")
                    load_w(stg, a2[:, d_, :], B_w, a2_d[d_], [64, 1024], "a2")
                load_w(stg, g1[:], B_w, g1_d.rearrange("(c p) e -> p c e", p=128), [128, 8, 128], "g1")
                load_w(stg, g2[:], B_w, g2_d, [128, 1024], "g2")
                S.barrier()

            if stop_after == "p1w":
                return finish([])
            xtm = Ring([(sbt(ph, "xtm%d" % i, [128, 1024], F32), Buf()) for i in range(2)])
            hx = sbt(ph, "hx", [128, 8, NT], F32)
            B_hx = Buf()
            xn = sbt(ph, "xn", [128, 8, NT], F32)
            B_xn = Buf()
            xx = sbt(ph, "xx", [128, 8, NT], F32)
            B_xx = Buf()
            sqt = sbt(ph, "sqt", [128, 8, NT], BF16)
            B_sq = Buf()
            rstd = sbt(ph, "rstd", [128, NT], F32)
            B_rstd = Buf()
            mix = [(sbt(ph, "mix%d" % i, [128, 8, NT], BF16), Buf()) for i in range(3)]
            lora = {k: (sbt(ph, "lo_" + k, [128, NT], BF16), Buf()) for k in ("w0", "w1", "a0", "a1", "g")}
            NW = 36
            wk_items = [(sbt(ph, "wk%d" % i, [128, NT], F32), Buf()) for i in range(NW)]
            wk = Ring(wk_items)
            wkb = Ring([(sbt(ph, "wkb%d" % i, [128, NT], BF16), Buf()) for i in range(14)])
            oblk = Ring([(sbt(ph, "oblk%d" % i, [128, 2, 2, 6, 128], BF16), Buf()) for i in range(2)])
            obuf2 = {id(b): Buf() for _, b in oblk.items}
            ovt = Ring([(sbt(ph, "ovt%d" % i, [128, 2, 128], BF16), Buf()) for i in range(2)])
            ogt = Ring([(sbt(ph, "ogt%d" % i, [128, NT], BF16), Buf()) for i in range(2)])
            obn = Ring([(sbt(ph, "obn%d" % i, [128, NT], F32), Buf()) for i in range(2)])
            pXr = Ring([(pst(ph, "pX%d" % i, [128, 4, 128], F32), PBuf()) for i in range(1)])
            pslots = pslots2(ph, "pp", 10)
            ptr_l = []
            for i_ in range(2):
                ptr_t = pst(ph, "ptr%d" % i_, [128, 4, 2, 128], BF16)
                bb_ = PBuf()
                ptr_l += [(ptr_t[:, i, :, :], bb_) for i in range(4)]
            ptr = Ring(ptr_l)

            for ti in range(17 if stop_after not in ("p1t0", "p1t1") else (1 if stop_after == "p1t0" else 2)):
                is_ctx = (ti == 0)
                wk = Ring(wk_items)
                src = ctx_d if is_ctx else x_d[(ti - 1) * NT: ti * NT, :]
                c0 = 0 if is_ctx else 2 + (ti - 1) * 2
                tok0 = 0 if is_ctx else (ti - 1) * NT
                rowlen = 256 if is_ctx else 64
                nrow = NT // rowlen
                for sub in range(2):
                    xt, bxt = xtm.next()
                    S.dma(dmaq.next(), xt[:], src[sub * 128:(sub + 1) * 128, :], writes=[bxt])
                    for half in range(2):
                        pX, B_pX = pXr.next()
                        for f4 in range(4):
                            fc = half * 4 + f4
                            S.op("pe", lambda e, fc=fc, f4=f4, xt=xt, pX=pX: e.transpose(pX[:, f4, :], xt[:, fc * 128:(fc + 1) * 128], identf),
                                 [bxt, B_cst], [B_pX], pe_accum=True)
                        copy("act" if half == 0 else "dve", hx[:, half * 4:(half + 1) * 4, sub * 128:(sub + 1) * 128], pX[:], [B_pX], [B_hx])
                if stop_after == "p1a":
                    return finish([])
                if not is_ctx:
                    S.dma("sp", hT_d[:, :, tok0:tok0 + NT], hx[:], reads=[B_hx], writes=[B_hT])
                pn, bpn = pslots.next()
                if is_ctx:
                    norm_mod(hx, B_hx, NT, xn, B_xn, lambda fc: der[:, 4, fc:fc + 1], lambda fc: sh_ap(0, 0, fc, 1),
                             sqt, B_sq, rstd, B_rstd, pn, bpn)
                else:
                    norm_mod(hx, B_hx, NT, xn, B_xn, lambda fc: sc_ap(0, 0, fc), lambda fc: sh_ap(0, 0, fc, 0),
                             sqt, B_sq, rstd, B_rstd, pn, bpn)
                xnr = xn[:].rearrange("p c (r t) -> p (c r) t", t=rowlen)
                xxr = xx[:].rearrange("p c (r t) -> p (c r) t", t=rowlen)
                R = rowlen
                S.op("dve", lambda e: e.tensor_tensor(out=xxr[:, :, 1:R - 1], in0=xnr[:, :, 0:R - 2], in1=xnr[:, :, 2:R], op=ALU.add),
                     [B_xn], [B_xx])
                S.op("pool", lambda e: e.tensor_copy(out=xxr[:, :, 0:1], in_=xnr[:, :, 1:2]), [B_xn], [B_xx])
                S.op("pool", lambda e: e.tensor_copy(out=xxr[:, :, R - 1:R], in_=xnr[:, :, R - 2:R - 1]), [B_xn], [B_xx])
                S.op("dve", lambda e: e.scalar_tensor_tensor(out=xx[:], in0=xx[:], scalar=0.5, in1=xn[:], op0=ALU.mult, op1=ALU.subtract),
                     [B_xn, B_xx], [B_xx])

                if stop_after == "p1b":
                    return finish([])

                def make_mix(p_, slot):
                    mt, bm = mix[slot]
                    for fc in range(8):
                        if fc % 2 == 0:
                            S.op("dve", lambda e, fc=fc: e.scalar_tensor_tensor(
                                out=mt[:, fc, :], in0=xx[:, fc, :], scalar=vecs[:, 4 + p_, fc:fc + 1], in1=xn[:, fc, :],
                                op0=ALU.mult, op1=ALU.add), [B_xx, B_xn, B_vecs], [bm])
                        else:
                            tmp_, btmp = wk.next()
                            S.op("act", lambda e, fc=fc, tmp_=tmp_: e.activation(
                                out=tmp_[:], in_=xx[:, fc, :], func=AF.Copy, scale=vecs[:, 4 + p_, fc:fc + 1]),
                                [B_xx, B_vecs], [btmp])
                            S.op("pool", lambda e, fc=fc, tmp_=tmp_: e.tensor_tensor(
                                out=mt[:, fc, :], in0=tmp_[:], in1=xn[:, fc, :], op=ALU.add), [btmp, B_xn], [bm])
                    return mt, bm

                def lora_hidden(mt, bm, wt, d_, ncol, key, func):
                    ps_, bps = pslots.next()
                    for dc in range(8):
                        lw_ = wt[:, d_, dc, :] if d_ is not None else wt[:, dc, :]
                        S.op("pe", lambda e, dc=dc, lw_=lw_: e.matmul(ps_[0:ncol, :], lhsT=lw_, rhs=mt[:, dc, :],
                                                                         start=(dc == 0), stop=(dc == 7)),
                             [bm, B_w], [bps], pe_accum=True)
                    lt, bl = lora[key]
                    S.op("act", lambda e: e.activation(out=lt[0:ncol, :], in_=ps_[0:ncol, :], func=func), [bps], [bl])

                mt, bm = make_mix(3, 0)
                lora_hidden(mt, bm, w1, 0, 64, "w0", AF.Tanh)
                lora_hidden(mt, bm, w1, 1, 64, "w1", AF.Tanh)
                mt, bm = make_mix(4, 1)
                lora_hidden(mt, bm, a1, 0, 64, "a0", AF.Copy)
                lora_hidden(mt, bm, a1, 1, 64, "a1", AF.Copy)
                mt, bm = make_mix(5, 2)
                lora_hidden(mt, bm, g1, None, 128, "g", AF.Sigmoid)
                if stop_after == "p1c":
                    return finish([])
                m_r, b_mr = make_mix(0, 0)
                m_k, b_mk = make_mix(1, 1)
                m_v, b_mv = make_mix(2, 2)

                S.barrier()
                wk = Ring(wk_items + [(t_[:, i_, :], Buf()) for t_ in (hx, xx, xn) for i_ in range(8)])

                def fc_task(fc):
                    _it = 0
                    fs = slice(fc * 128, (fc + 1) * 128)


                    def proj(mt_, bm_, p_):
                        ps_, bps = pslots.next()
                        for dc in range(8):
                            S.op("pe", lambda e, dc=dc: e.matmul(ps_[:], lhsT=wrkv[:, p_, dc, fs], rhs=mt_[:, dc, :],
                                                                  start=(dc == 0), stop=(dc == 7)),
                                 [bm_, B_w], [bps], pe_accum=True)
                        return ps_, bps

                    def small_proj(wt2, d_, key, ncol):
                        ps_, bps = pslots.next()
                        lt, bl = lora[key]
                        lhs = wt2[:, d_, fs] if d_ is not None else wt2[:, fs]
                        S.op("pe", lambda e: e.matmul(ps_[:], lhsT=lhs, rhs=lt[0:ncol, :], start=True, stop=True),
                             [bl, B_w], [bps], pe_accum=True)
                        return ps_, bps

                    p_r, b_pr = proj(m_r, b_mr, 0)
                    p_k, b_pk = proj(m_k, b_mk, 1)
                    p_v, b_pv = proj(m_v, b_mv, 2)
                    if stop_after == "p2mm" and _it == 1:
                        S.final_wait("act", [b_pr, b_pk, b_pv])
                        return finish([B_vtm, B_blk])
                    r_t, b_r = wk.next()
                    k_t, b_k = wk.next()
                    v_t, b_v = wk.next()
                    copy("act", r_t[:], p_r[:], [b_pr], [b_r])
                    copy("dve", k_t[:], p_k[:], [b_pk], [b_k])
                    copy("act", v_t[:], p_v[:], [b_pv], [b_v])
                    yield
                    kk_t, b_kk = wk.next()
                    S.op("act", lambda e: e.activation(out=kk_t[:], in_=k_t[:], func=AF.Copy, scale=vecs[:, 14, fc:fc + 1]),
                         [b_k, B_vecs], [b_kk])
                    sq_, b_sq_ = wkb.next()
                    S.op("act", lambda e: e.activation(out=sq_[:], in_=kk_t[:], func=AF.Square), [b_kk], [b_sq_])
                    pn2, bpn2 = pslots.next()
                    S.op("pe", lambda e: e.matmul(pn2[:], lhsT=blk64_b, rhs=sq_[:], start=True, stop=True), [b_sq_, B_cstb], [bpn2])
                    rn_t, b_rn = wk.next()
                    S.op("act", lambda e: e.activation(out=rn_t[:], in_=pn2[:], func=AF.Sqrt), [bpn2], [b_rn])
                    S.op("dve", lambda e: e.tensor_scalar(out=rn_t[:], in0=rn_t[:], scalar1=1e-12, scalar2=None, op0=ALU.max), [b_rn], [b_rn])
                    S.op("dve", lambda e: e.reciprocal(out=rn_t[:], in_=rn_t[:]), [b_rn], [b_rn])
                    S.op("pool", lambda e: e.tensor_tensor(out=kk_t[:], in0=kk_t[:], in1=rn_t[:], op=ALU.mult), [b_kk, b_rn], [b_kk])
                    yield
                    if not is_ctx:
                        p_g, b_pg = small_proj(g2, None, "g", 128)
                        og, bog = ogt.next()
                        copy("act", og[:], p_g[:], [b_pg], [bog])
                        S.dma("sp", gT_d[:, fc, tok0:tok0 + NT], og[:], reads=[bog], writes=[B_gT])
                    yield
                    vb_, b_vb = wkb.next()
                    copy("pool", vb_[:], v_t[:], [b_v], [b_vb])
                    pt_, bpt = ptr.next()
                    for sub in range(2):
                        S.op("pe", lambda e, sub=sub: e.transpose(pt_[:, sub, :], vb_[:, sub * 128:(sub + 1) * 128], ident_b),
                             [b_vb, B_cstb], [bpt], pe_accum=True)
                    ov, bov = ovt.next()
                    copy("dve", ov[:], pt_[:], [bpt], [bov])
                    S.dma("sp", vtm_d[c0:c0 + 2, :, fs].rearrange("c p e -> p c e"), ov[:], reads=[bov], writes=[B_vtm])

                    yield
                    ob, bob0 = oblk.next()
                    bobs = [bob0, obuf2[id(bob0)]]
                    kds = [None, None]

                    def dir_task(d_):
                        bob = bobs[d_]
                        p_w, b_pw = small_proj(w2, d_, "w%d" % d_, 64)
                        p_a, b_pa = small_proj(a2, d_, "a%d" % d_, 64)
                        sg_t, b_sg = wk.next()
                        S.op("act", lambda e: e.activation(out=sg_t[:], in_=p_w[:], func=AF.Sigmoid, bias=vecs[:, 10 + d_, fc:fc + 1], scale=1.0),
                             [b_pw, B_vecs], [b_sg])
                        a_t, b_a = wk.next()
                        S.op("act", lambda e: e.activation(out=a_t[:], in_=p_a[:], func=AF.Sigmoid, bias=vecs[:, 12 + d_, fc:fc + 1], scale=1.0),
                             [b_pa, B_vecs], [b_a])
                        yield
                        lw_t, b_lw = wk.next()
                        S.op("act", lambda e: e.activation(out=lw_t[:], in_=sg_t[:], func=AF.Copy, scale=-DECAY_C),
                             [b_sg], [b_lw])
                        kka, b_kka = wk.next()
                        S.op("pool", lambda e: e.tensor_tensor(out=kka[:], in0=kk_t[:], in1=a_t[:], op=ALU.mult), [b_kk, b_a], [b_kka])
                        kd, b_kd = wk.next()
                        S.op("act", lambda e: e.activation(out=kd[:], in_=a_t[:], func=AF.Identity, scale=vecs[:, 15, fc:fc + 1], bias=omk[:, fc:fc + 1]),
                             [b_a, B_vecs, B_omk], [b_kd])
                        S.op("pool", lambda e: e.tensor_tensor(out=kd[:], in0=kd[:], in1=k_t[:], op=ALU.mult),
                             [b_kd, b_k], [b_kd])
                        kds[d_] = (kd, b_kd)
                        yield
                        L_t, b_L = wk.next()
                        S.op("dve", lambda e: e.tensor_tensor_scan(out=L_t[:], data0=scanmask, data1=lw_t[:], initial=0.0,
                                                                   op0=ALU.mult, op1=ALU.add), [b_lw, B_cst], [b_L])
                        yield
                        Lx_t, b_Lx = wk.next()
                        S.op("pool", lambda e: e.tensor_tensor(out=Lx_t[:], in0=L_t[:], in1=lw_t[:], op=ALU.subtract), [b_L, b_lw], [b_Lx])
                        D_t, b_D = wk.next()
                        L3 = L_t[:].rearrange("p (c t) -> p c t", t=128)
                        Tot = L3[:, :, 127:128]
                        S.op("dve", lambda e: e.tensor_tensor(out=D_t[:].rearrange("p (c t) -> p c t", t=128),
                                                              in0=Tot.broadcast_to([128, 2, 128]), in1=L3, op=ALU.subtract),
                             [b_L], [b_D])
                        S.op("act", lambda e: e.activation(out=gam[:, d_, c0:c0 + 2, fc], in_=L3[:, :, 127], func=AF.Exp),
                             [b_L], [B_gam])
                        yield
                        ea, b_ea = wk.next()
                        eb, b_eb = wk.next()
                        er, b_er = wk.next()
                        eh, b_eh = wk.next()
                        if d_ == 0:
                            S.op("act", lambda e: e.activation(out=ea[:], in_=Lx_t[:], func=AF.Exp), [b_Lx], [b_ea])
                            S.op("act", lambda e: e.activation(out=eb[:], in_=L_t[:], func=AF.Exp, scale=-1.0), [b_L], [b_eb])
                            S.op("act", lambda e: e.activation(out=er[:], in_=L_t[:], func=AF.Exp), [b_L], [b_er])
                            S.op("act", lambda e: e.activation(out=eh[:], in_=D_t[:], func=AF.Exp), [b_D], [b_eh])
                        else:
                            Dl, b_Dl = wk.next()
                            S.op("pool", lambda e: e.tensor_tensor(out=Dl[:], in0=D_t[:], in1=lw_t[:], op=ALU.add), [b_D, b_lw], [b_Dl])
                            S.op("act", lambda e: e.activation(out=ea[:], in_=D_t[:], func=AF.Exp), [b_D], [b_ea])
                            S.op("act", lambda e: e.activation(out=eb[:], in_=Dl[:], func=AF.Exp, scale=-1.0), [b_Dl], [b_eb])
                            S.op("act", lambda e: e.activation(out=er[:], in_=Dl[:], func=AF.Exp), [b_Dl], [b_er])
                            S.op("act", lambda e: e.activation(out=eh[:], in_=Lx_t[:], func=AF.Exp), [b_Lx], [b_eh])
                        yield

                        def o4(q):
                            return ob[:, d_, :, q, :]

                        def v3(t_):
                            return t_[:].rearrange("p (c t) -> p c t", t=128)
                        S.op("dve", lambda e: e.scalar_tensor_tensor(out=o4(0), in0=v3(kk_t), scalar=-1.0, in1=v3(ea), op0=ALU.mult, op1=ALU.mult),
                             [b_kk, b_ea], [bob])
                        S.op("pool", lambda e: e.tensor_tensor(out=o4(1), in0=v3(r_t), in1=v3(er), op=ALU.mult), [b_r, b_er], [bob])
                        S.op("dve", lambda e: e.tensor_tensor(out=o4(2), in0=v3(kka), in1=v3(eb), op=ALU.mult), [b_kka, b_eb], [bob])
                        S.op("pool", lambda e: e.tensor_tensor(out=o4(3), in0=v3(kd), in1=v3(eb), op=ALU.mult), [b_kd, b_eb], [bob])
                        yield
                        for q, srct, bsrc in ((4, kka, b_kka), (5, kd, b_kd)):
                            hb, b_hb = wkb.next()
                            S.op("dve" if q == 4 else "pool", lambda e, srct=srct, hb=hb: e.tensor_tensor(out=hb[:], in0=srct[:], in1=eh[:], op=ALU.mult),
                                 [bsrc, b_eh], [b_hb])
                            pt2, bpt2 = ptr.next()
                            for sub in range(2):
                                S.op("pe", lambda e, sub=sub, hb=hb, pt2=pt2: e.transpose(pt2[:, sub, :], hb[:, sub * 128:(sub + 1) * 128], ident_b),
                                     [b_hb, B_cstb], [bpt2], pe_accum=True)
                            copy("act" if q == 4 else "dve", ob[:, d_, :, q, :], pt2[:], [bpt2], [bob])
                            yield
                        dst = blk_d[d_, c0:c0 + 2, :, fc * 768:(fc + 1) * 768].rearrange("c p (q t) -> p c q t", q=6)
                        S.dma("sp", dst, ob[:, d_, :, :, :], reads=[bob], writes=[B_blk])

                    yield from lockstep_gen([dir_task(0), dir_task(1)])
                    if not is_ctx:
                        (kd0, b_kd0), (kd1, b_kd1) = kds
                        S.op("pool", lambda e: e.tensor_tensor(out=kd1[:], in0=kd1[:], in1=kd0[:], op=ALU.add), [b_kd1, b_kd0], [b_kd1])
                        pb_, b_pb = wkb.next()
                        S.op("dve", lambda e: e.scalar_tensor_tensor(out=pb_[:], in0=r_t[:], scalar=vecs[:, 16, fc:fc + 1], in1=kd1[:],
                                                                     op0=ALU.mult, op1=ALU.mult), [b_r, b_kd1, B_vecs], [b_pb])
                        pn3, bpn3 = pslots.next()
                        S.op("pe", lambda e: e.matmul(pn3[:], lhsT=blk64_b, rhs=pb_[:], start=True, stop=True), [b_pb, B_cstb], [bpn3])
                        obn_, bobn = obn.next()
                        S.op("dve", lambda e: e.tensor_tensor(out=obn_[:], in0=pn3[:], in1=v_t[:], op=ALU.mult), [bpn3, b_v], [bobn])
                        S.dma("sp", bon_d[:, fc, tok0:tok0 + NT], obn_[:], reads=[bobn], writes=[B_bon])

                for fa in range(0, 8, 2):
                    run_lockstep([fc_task(fa), fc_task(fa + 1)])
                S.barrier()
            S.barrier()

        if dbg and "d_gam" in dbg_d:
            S.dma("sp", dbg_d["d_gam"], gam[:].rearrange("p a b c -> p (a b c)"), reads=[B_gam], writes=[B_dbg["d_gam"]])
        if stop_after in ("phase1", "p1t0", "p1t1"):
            return finish([B_hT, B_blk, B_vtm, B_gT, B_bon])

        with contextlib.ExitStack() as ph:
            St = sbt(ph, "St", [128, 2, 8, 64], F32)
            Sb = sbt(ph, "Sb", [128, 2, 8, 64], BF16)
            B_S = [[Buf() for _ in range(4)] for _ in range(2)]
            S.op("dve", lambda e: e.memset(St[:], 0.0), [], [b for bb in B_S for b in bb])
            S.op("pool", lambda e: e.memset(Sb[:], 0.0), [], [b for bb in B_S for b in bb])
            blkin = [Ring([(sbt(ph, "bi%d_%d" % (d_, i), [128, 8, 6, 128], BF16), Buf()) for i in range(2)]) for d_ in range(2)]
            vin = [Ring([(sbt(ph, "vi%d_%d" % (d_, i), [128, 1024], BF16), Buf()) for i in range(2)]) for d_ in range(2)]
            NG = 3
            gctx = Ring([dict(
                AB=sbt(ph, "AB%d" % i, [128, 4, 256], BF16), AK=sbt(ph, "AK%d" % i, [128, 4, 256], BF16),
                A32=sbt(ph, "A32%d" % i, [128, 4, 128], F32),
                AT=sbt(ph, "AT%d" % i, [128, 4, 128], F32),
                PM=[sbt(ph, "PM%d_%d" % (i, j), [128, 4, 256], F32) for j in range(2)],
                PT=[sbt(ph, "PT%d_%d" % (i, j), [128, 4, 128], F32) for j in range(2)],
                MI=sbt(ph, "MI%d" % i, [128, 4, 128], BF16),
                W=sbt(ph, "W%d" % i, [128, 4, 64], BF16), U=sbt(ph, "U%d" % i, [128, 4, 64], BF16),
                b={k: Buf() for k in ("AB", "A32", "AK", "AT", "PM0", "PM1", "PT0", "PT1", "MI", "W", "U")})
                for i in range(NG)])
            ysb = [Ring([(sbt(ph, "ys%d_%d" % (d_, i), [128, 8, 128], F32), Buf()) for i in range(2)]) for d_ in range(2)]
            PAg = [(pst(ph, "PA%d" % i, [128, 4, 256], F32), PBuf()) for i in range(2)]
            PTg = [(pst(ph, "PTg%d" % i, [128, 4, 128], F32), PBuf()) for i in range(2)]
            PH = [pst(ph, "PH%d" % i, [128, 512], F32) for i in range(2)]
            B_PH = PBuf()
            PWv = [PH[h][:, 0:128].rearrange("p (j v) -> p j v", j=2) for h in range(2)]
            PUv = [PH[h][:, 128:256].rearrange("p (j v) -> p j v", j=2) for h in range(2)]
            PYv = PH[0][:, 256:512].rearrange("p (j t) -> p j t", j=2)
            PSv = PH[1][:, 256:384].rearrange("p (j v) -> p j v", j=2)

            order = [list(range(NCH)), [1, 0] + list(range(NCH - 1, 1, -1))]
            for step in range(NCH):
                cur = []
                for d_ in range(2):
                    c = order[d_][step]
                    bt, bb = blkin[d_].next()
                    S.dma("sp", bt[:].rearrange("p f q t -> p (f q t)"), blk_d[d_, c], reads=[B_blk], writes=[bb])
                    vt, bv = vin[d_].next()
                    S.dma("sp" if d_ == 0 else "sp", vt[:], vtm_d[c], reads=[B_vtm], writes=[bv])
                    cur.append((c, bt, bb, vt, bv))
                if stop_after == "s_0":
                    S.final_wait("act", [cur[0][2], cur[0][4], cur[1][2], cur[1][4]] + [b for bb in B_S for b in bb]); return finish([])
                ys = [None, None]
                for d_ in range(2):
                    if cur[d_][0] >= 2:
                        ys[d_] = ysb[d_].next()
                def grp(q, d_):
                    c, bt, bb, vt, bv = cur[d_]
                    G = gctx.next()
                    gb = G["b"]
                    items = [(j, hh) for hh in range(2) for j in range(2)]
                    H2 = (slice(0, 2), slice(2, 4))
                    pa, bpa = PAg[d_]
                    ptp, bptp = PTg[d_]
                    mb = MB[d_].unsqueeze(1).broadcast_to([128, 4, 256])
                    mtm = MT[d_].unsqueeze(1).broadcast_to([128, 4, 128])

                    def a_mm(q_lhs, rhs_fn, out_fn):
                        for i, (j, hh) in enumerate(items):
                            fc = 2 * q + j
                            hs = slice(hh * 64, hh * 64 + 64)
                            S.op("pe", lambda e, i=i, fc=fc, hs=hs: e.matmul(out_fn(i), lhsT=bt[hs, fc, q_lhs, :], rhs=rhs_fn(hs, fc), start=True, stop=True),
                                 [bb], [bpa], pe_accum=True)
                    a_mm(2, lambda hs, fc: bt[hs, fc, 0:2, :], lambda i: pa[:, i, :])
                    for h2 in H2:
                        S.op("dve", lambda e, h2=h2: e.tensor_tensor(out=G["AB"][:, h2, :], in0=pa[:, h2, :], in1=mb[:, h2, :], op=ALU.mult), [bpa, B_cst], [gb["AB"]])
                    for h2 in H2:
                        S.op("act", lambda e, h2=h2: e.activation(out=G["A32"][:, h2, :], in_=pa[:, h2, 0:128], func=AF.Copy), [bpa], [gb["A32"]])
                    S.op("pool", lambda e: e.tensor_tensor(out=G["A32"][:], in0=G["A32"][:], in1=mb[:, :, 0:128], op=ALU.mult), [gb["A32"], B_cst], [gb["A32"]])
                    S.op("pool", lambda e: e.tensor_tensor(out=G["PM"][1][:, :, 128:256], in0=G["A32"][:],
                                                           in1=identf.unsqueeze(1).broadcast_to([128, 4, 128]), op=ALU.add),
                         [gb["A32"], B_cst], [gb["PM1"]])
                    yield
                    a_mm(3, lambda hs, fc: bt[hs, fc, 0:2, :], lambda i: pa[:, i, :])
                    for h2 in H2:
                        S.op("dve", lambda e, h2=h2: e.tensor_tensor(out=G["AK"][:, h2, :], in0=pa[:, h2, :], in1=mb[:, h2, :], op=ALU.mult), [bpa, B_cst], [gb["AK"]])
                    yield
                    a_mm(0, lambda hs, fc: bt[hs, fc, 2, :], lambda i: pa[:, i, 0:128])
                    for h2 in H2:
                        S.op("dve", lambda e, h2=h2: e.tensor_tensor(out=G["AT"][:, h2, :], in0=pa[:, h2, 0:128], in1=mtm[:, h2, :], op=ALU.mult), [bpa, B_cst], [gb["AT"]])
                    yield
                    for i in range(4):
                        S.op("pe", lambda e, i=i: e.matmul(pa[:, i, 0:128], lhsT=G["AT"][:, i, :], rhs=G["A32"][:, i, :], start=True, stop=True),
                             [gb["AT"], gb["A32"]], [bpa], pe_accum=True)
                    for i in range(4):
                        S.op("pe", lambda e, i=i: e.matmul(ptp[:, i, :], lhsT=G["A32"][:, i, :], rhs=G["AT"][:, i, :], start=True, stop=True),
                             [gb["AT"], gb["A32"]], [bptp], pe_accum=True)
                    for h2 in H2:
                        S.op("act", lambda e, h2=h2: e.activation(out=G["PM"][1][:, h2, 0:128], in_=pa[:, h2, 0:128], func=AF.Copy), [bpa], [gb["PM1"]])
                    S.op("dve", lambda e: e.tensor_copy(out=G["PT"][1][:], in_=ptp[:]), [bptp], [gb["PT1"]])
                    yield
                    for k in range(1, 6):
                        cu, nx = k % 2, (k + 1) % 2
                        PMc, PTc, PMn, PTn = G["PM"][cu], G["PT"][cu], G["PM"][nx], G["PT"][nx]
                        bPMc, bPTc, bPMn, bPTn = gb["PM%d" % cu], gb["PT%d" % cu], gb["PM%d" % nx], gb["PT%d" % nx]
                        last = (k == 5)
                        for i in range(4):
                            rhs_ = PMc[:, i, 128:256] if last else PMc[:, i, :]
                            out_ = pa[:, i, 128:256] if last else pa[:, i, :]
                            S.op("pe", lambda e, i=i, rhs_=rhs_, out_=out_, PTc=PTc: e.matmul(out_, lhsT=PTc[:, i, :], rhs=rhs_, start=True, stop=True),
                                 [bPMc, bPTc], [bpa], pe_accum=True)
                        for i in range(4):
                            S.op("pe", lambda e, i=i, PMc=PMc, PTc=PTc: e.matmul(ptp[:, i, :], lhsT=PMc[:, i, 0:128], rhs=PTc[:, i, :], start=True, stop=True),
                                 [bPMc, bPTc], [bptp], pe_accum=True)
                        if not last:
                            for h2 in H2:
                                S.op("act", lambda e, PMn=PMn, h2=h2: e.activation(out=PMn[:, h2, 0:128], in_=pa[:, h2, 0:128], func=AF.Copy), [bpa], [bPMn])
                        for h2 in H2:
                            S.op("dve", lambda e, PMn=PMn, PMc=PMc, h2=h2: e.tensor_tensor(out=PMn[:, h2, 128:256], in0=pa[:, h2, 128:256], in1=PMc[:, h2, 128:256], op=ALU.add),
                                 [bpa, bPMc], [bPMn])
                        S.op("act" if k % 2 else "dve", (lambda e, PTn=PTn: e.activation(out=PTn[:], in_=ptp[:], func=AF.Copy)) if k % 2 else (lambda e, PTn=PTn: e.tensor_copy(out=PTn[:], in_=ptp[:])),
                             [bptp], [bPTn])
                        yield
                    PM6, PT6 = G["PM"][0], G["PT"][0]
                    for i in range(4):
                        S.op("pe", lambda e, i=i: e.matmul(pa[:, i, 128:256], lhsT=PT6[:, i, :], rhs=PM6[:, i, 128:256], start=True, stop=True),
                             [gb["PM0"], gb["PT0"]], [bpa], pe_accum=True)
                    for h2 in H2:
                        S.op("dve", lambda e, h2=h2: e.tensor_tensor(out=G["MI"][:, h2, :], in0=pa[:, h2, 128:256], in1=PM6[:, h2, 128:256], op=ALU.add),
                             [bpa, gb["PM0"]], [gb["MI"]])
                    yield
                    bS = B_S[d_][q]
                    for i, (j, hh) in enumerate(items):
                        fc = 2 * q + j
                        hs = slice(hh * 64, hh * 64 + 64)
                        S.op("pe", lambda e, i=i, fc=fc, hs=hs: e.matmul(PWv[i // 2][:, i % 2, :], lhsT=bt[hs, fc, 0, :], rhs=Sb[hs, d_, fc, :], start=True, stop=False),
                             [bb, bS], [B_PH], pe_accum=True)
                        S.op("pe", lambda e, i=i, fc=fc, hh=hh: e.matmul(PWv[i // 2][:, i % 2, :], lhsT=G["AK"][:, i, 0:128], rhs=vt[:, fc * 128 + hh * 64: fc * 128 + hh * 64 + 64],
                                                                        start=False, stop=True),
                             [gb["AK"], bv], [B_PH], pe_accum=True)
                    for h_ in range(2):
                        S.op("act", lambda e, h_=h_: e.activation(out=G["W"][:, 2 * h_:2 * h_ + 2, :], in_=PWv[h_], func=AF.Copy), [B_PH], [gb["W"]])
                    yield
                    for i in range(4):
                        S.op("pe", lambda e, i=i: e.matmul(PUv[i // 2][:, i % 2, :], lhsT=G["MI"][:, i, :], rhs=G["W"][:, i, :], start=True, stop=True),
                             [gb["MI"], gb["W"]], [B_PH], pe_accum=True)
                    for h_ in range(2):
                        S.op("act", lambda e, h_=h_: e.activation(out=G["U"][:, 2 * h_:2 * h_ + 2, :], in_=PUv[h_], func=AF.Copy), [B_PH], [gb["U"]])
                    yield
                    for i, (j, hh) in enumerate(items):
                        fc = 2 * q + j
                        hs = slice(hh * 64, hh * 64 + 64)
                        vsl = vt[:, fc * 128 + hh * 64: fc * 128 + hh * 64 + 64]
                        if c >= 2:
                            S.op("pe", lambda e, fc=fc, hs=hs, j=j: e.matmul(PYv[hs, j, :], lhsT=Sb[hs, d_, fc, :], rhs=bt[hs, fc, 1, :], start=True, stop=False),
                                 [bb, bS], [B_PH], pe_accum=True)
                            S.op("pe", lambda e, i=i, hs=hs, j=j: e.matmul(PYv[hs, j, :], lhsT=G["U"][:, i, :], rhs=G["AB"][:, i, 128:256], start=False, stop=False),
                                 [gb["U"], gb["AB"]], [B_PH], pe_accum=True)
                            S.op("pe", lambda e, i=i, hs=hs, j=j, vsl=vsl: e.matmul(PYv[hs, j, :], lhsT=vsl, rhs=G["AK"][:, i, 128:256], start=False, stop=True),
                                 [bv, gb["AK"]], [B_PH], pe_accum=True)
                        S.op("pe", lambda e, i=i, fc=fc, hs=hs, j=j: e.matmul(PSv[hs, j, :], lhsT=bt[:, fc, 4, hs], rhs=G["U"][:, i, :], start=True, stop=False),
                             [bb, gb["U"]], [B_PH], pe_accum=True)
                        S.op("pe", lambda e, fc=fc, hs=hs, j=j, vsl=vsl: e.matmul(PSv[hs, j, :], lhsT=bt[:, fc, 5, hs], rhs=vsl, start=False, stop=True),
                             [bb, bv], [B_PH], pe_accum=True)
                    if c >= 2:
                        yt_, byt = ys[d_]
                        S.op("act", lambda e: e.activation(out=yt_[:, 2 * q:2 * q + 2, :], in_=PYv, func=AF.Copy), [B_PH], [byt])
                    for j in range(2):
                        fc = 2 * q + j
                        S.op("dve", lambda e, fc=fc, j=j: e.scalar_tensor_tensor(out=St[:, d_, fc, :], in0=St[:, d_, fc, :], scalar=gam[:, d_, c, fc:fc + 1],
                                                                                 in1=PSv[:, j, :], op0=ALU.mult, op1=ALU.add),
                             [B_PH, bS, B_gam], [bS])
                    S.op("pool", lambda e: e.tensor_copy(out=Sb[:, d_, 2 * q:2 * q + 2, :], in_=St[:, d_, 2 * q:2 * q + 2, :]), [bS], [bS])
                    yield

                for q in range(4):
                    run_lockstep([grp(q, 0), grp(q, 1)])
                if stop_after == "s_g" or (stop_after == "s_h" and step == 2):
                    S.final_wait("sp", [b for bb in B_S for b in bb]); return finish([])
                for d_ in range(2):
                    c = cur[d_][0]
                    if c >= 2:
                        yt_, byt = ys[d_]
                        S.dma("sp", yT_d[d_, :, :, (c - 2) * 128:(c - 1) * 128], yt_[:], reads=[byt], writes=[B_yT])
            S.barrier()
        if stop_after == "scan":
            return finish([B_yT, B_hT])

        def ring_tiles(ph, name, shape, dt, n):
            return Ring([(sbt(ph, "%s%d" % (name, i), shape, dt), Buf()) for i in range(n)])

        with contextlib.ExitStack() as ph:
            wo = sbt(ph, "wo", [128, 8, 1024], BF16)
            B_wo = Buf()
            with contextlib.ExitStack() as ph2:
                stg = Ring([(sbt(ph2, "stgr%d" % i, [128, 8192], F32), Buf()) for i in range(1)])
                load_w(stg, wo[:], B_wo, wo_d.rearrange("(c p) e -> p c e", p=128), [128, 8, 1024], "wo")
                S.barrier()
            y0r = ring_tiles(ph, "y0r", [128, 8, NT], F32, 2)
            y1r = ring_tiles(ph, "y1r", [128, 8, NT], F32, 2)
            bnr = ring_tiles(ph, "bnr", [128, 8, NT], F32, 2)
            gtr = ring_tiles(ph, "gtr", [128, 8, NT], BF16, 2)
            hr = ring_tiles(ph, "hr", [128, 8, NT], F32, 2)
            zb = ring_tiles(ph, "zb", [128, 8, NT], BF16, 2)
            wkr = ring_tiles(ph, "wkr", [128, NT], F32, 8)
            wkbr = ring_tiles(ph, "wkbr", [128, NT], BF16, 4)
            psr = pslots2(ph, "psr", 8)
            def ro_tile(ti):
                tsl = slice(ti * NT, (ti + 1) * NT)
                y0, by0 = y0r.next()
                y1, by1 = y1r.next()
                bn, bbn = bnr.next()
                gt, bgt = gtr.next()
                ht, bht = hr.next()
                S.dma("sp", y0[:], yT_d[0, :, :, tsl], reads=[B_yT], writes=[by0])
                S.dma("sp", y1[:], yT_d[1, :, :, tsl], reads=[B_yT], writes=[by1])
                S.dma("sp", bn[:], bon_d[:, :, tsl], reads=[B_bon], writes=[bbn])
                S.dma("sp", gt[:], gT_d[:, :, tsl], reads=[B_gT], writes=[bgt])
                S.dma("sp", ht[:], hT_d[:, :, tsl], reads=[B_hT], writes=[bht])
                S.op("pool", lambda e: e.tensor_tensor(out=y0[:], in0=y0[:], in1=y1[:], op=ALU.add), [by0, by1], [by0])
                S.op("pool", lambda e: e.tensor_tensor(out=y0[:], in0=y0[:], in1=bn[:], op=ALU.add), [by0, bbn], [by0])
                z, bz = zb.next()
                yield
                for fc in range(8):
                    yb, byb = wkbr.next()
                    copy("act", yb[:], y0[:, fc, :], [by0], [byb])
                    pmn, bpmn = psr.next()
                    S.op("pe", lambda e: e.matmul(pmn[:], lhsT=blk64_b, rhs=yb[:], start=True, stop=True), [byb, B_cstb], [bpmn])
                    dd, bdd = wkr.next()
                    S.op("dve", lambda e: e.scalar_tensor_tensor(out=dd[:], in0=pmn[:], scalar=-1.0 / 64, in1=y0[:, fc, :], op0=ALU.mult, op1=ALU.add),
                         [bpmn, by0], [bdd])
                    yield
                    sqb, bsqb = wkbr.next()
                    S.op("act", lambda e: e.activation(out=sqb[:], in_=dd[:], func=AF.Square), [bdd], [bsqb])
                    pvr, bpvr = psr.next()
                    S.op("pe", lambda e: e.matmul(pvr[:], lhsT=blk64_b, rhs=sqb[:], start=True, stop=True), [bsqb, B_cstb], [bpvr])
                    yield
                    rs, brs = wkr.next()
                    S.op("act", lambda e: e.activation(out=rs[:], in_=pvr[:], func=AF.Sqrt, bias=epsc[:, 1:2], scale=1.0 / 64), [bpvr, B_epsc], [brs])
                    S.op("dve", lambda e: e.reciprocal(out=rs[:], in_=rs[:]), [brs], [brs])
                    S.op("pool", lambda e: e.tensor_tensor(out=dd[:], in0=dd[:], in1=rs[:], op=ALU.mult), [bdd, brs], [bdd])
                    S.op("act", lambda e: e.activation(out=dd[:], in_=dd[:], func=AF.Identity, bias=vecs[:, 18, fc:fc + 1], scale=vecs[:, 17, fc:fc + 1]),
                         [bdd, B_vecs], [bdd])
                    S.op("pool", lambda e: e.tensor_tensor(out=z[:, fc, :], in0=dd[:], in1=gt[:, fc, :], op=ALU.mult), [bdd, bgt], [bz])
                for fo in range(8):
                    yield
                    po, bpo = psr.next()
                    for dc in range(8):
                        S.op("pe", lambda e, dc=dc: e.matmul(po[:], lhsT=wo[:, dc, fo * 128:(fo + 1) * 128], rhs=z[:, dc, :], start=(dc == 0), stop=(dc == 7)),
                             [bz, B_wo], [bpo], pe_accum=True)
                    S.op("dve", lambda e: e.scalar_tensor_tensor(out=ht[:, fo, :], in0=po[:], scalar=gt_ap(0, 0, fo), in1=ht[:, fo, :], op0=ALU.mult, op1=ALU.add),
                         [bpo, bht, B_mod], [bht])
                S.dma("sp", hT_d[:, :, tsl], ht[:], reads=[bht], writes=[B_hT])
            for t2 in range(0, T // NT, 2):
                run_lockstep([ro_tile(t2), ro_tile(t2 + 1)])
            S.barrier()

        if stop_after == "readout":
            return finish([B_hT])

        def moe_phase(l):
            TT = 1024
            NS = TT // NT
            NX = 512
            NSX = TT // NX
            with contextlib.ExitStack() as ph:
                wgu = ring_tiles(ph, "wgu", [128, 2, 8, 512], BF16, 2)
                wdn = ring_tiles(ph, "wdn", [128, 4, 1024], BF16, 2)
                stg = ring_tiles(ph, "stgm", [128, 2048], F32, 4)
                xm = sbt(ph, "xm", [128, 8, TT], BF16)
                B_xm = Buf()
                acc = sbt(ph, "acc", [128, 8, TT], F32)
                B_acc = [Buf() for _ in range(NSX)]
                hin = ring_tiles(ph, "hin", [128, 8, NT], F32, 2)
                xm32 = ring_tiles(ph, "xm32", [128, 8, NT], F32, 1)
                sqt = sbt(ph, "sqm", [128, 8, NT], BF16)
                B_sq = Buf()
                rstd = sbt(ph, "rstdm", [128, NT], F32)
                B_rstd = Buf()
                sc_all = sbt(ph, "sc_all", [128, 8, 16], F32)
                B_sc = Buf()
                rt = {k: (sbt(ph, "rt_" + k, [128, 8, 16], F32), Buf()) for k in ("sel", "m1", "eq", "m2", "gs", "gsel", "msk", "gate", "comb")}
                small = {k: (sbt(ph, "sm_" + k, [128, 8, 4], F32), Buf()) for k in ("m1", "m2", "gs", "gsel")}
                small1 = {k: (sbt(ph, "s1_" + k, [128, 8], F32), Buf()) for k in ("gmax", "den")}
                combT = sbt(ph, "combT", [16, TT], BF16)
                B_combT = Buf()
                cbs = ring_tiles(ph, "cbs", [128, NX], F32, 2)
                sgs = ring_tiles(ph, "sgs", [128, NX], F32, 3)
                t1s = ring_tiles(ph, "t1s", [128, NX], F32, 3)
                hes = ring_tiles(ph, "hes", [128, 4, NX], BF16, 2)
                pgu = pslots2(ph, "pgu", 4, width=512)
                pdn = pslots2(ph, "pdn", 3, width=512)
                pms = pslots2(ph, "pms", 2)

                pending = []
                resid_prev = None
                for tt in range(T // TT):
                    for sub in range(NS):
                        tsl = slice(tt * TT + sub * NT, tt * TT + (sub + 1) * NT)
                        ht, bht = hin.next()
                        S.dma(dmaq.next(), ht[:], hT_d[:, :, tsl], reads=[B_hT], writes=[bht])
                        x32, bx32 = xm32.next()
                        pn, bpn = pms.next()
                        norm_mod(ht, bht, NT, x32, bx32, lambda fc: sc_ap(l, 1, fc), lambda fc: sh_ap(l, 1, fc, 0),
                                 sqt, B_sq, rstd, B_rstd, pn, bpn)
                        copy("pool", xm[:, :, sub * NT:(sub + 1) * NT], x32[:], [bx32], [B_xm])
                        pr, bpr = pms.next()
                        for s2 in range(2):
                            for dc in range(8):
                                S.op("pe", lambda e, s2=s2, dc=dc: e.matmul(pr[:, s2 * 16:(s2 + 1) * 16], lhsT=x32[:, dc, s2 * 128:(s2 + 1) * 128], rhs=rwt[:, dc, :],
                                                                            start=(dc == 0), stop=(dc == 7)),
                                     [bx32, B_rwt], [bpr], pe_accum=True)
                        S.op("act", lambda e, sub=sub: e.activation(out=sc_all[:, 2 * sub:2 * sub + 2, :], in_=pr[:, 0:32].rearrange("p (a b) -> p a b", a=2), func=AF.Sigmoid),
                             [bpr], [B_sc])
                    sel, bsel = rt["sel"]
                    S.op("dve", lambda e: e.tensor_tensor(out=sel[:], in0=sc_all[:], in1=rtb[:].unsqueeze(1).broadcast_to([128, 8, 16]), op=ALU.add),
                         [B_sc, B_rtb], [bsel])
                    sel4 = sel[:].rearrange("p s (g j) -> p (s g) j", j=4)
                    m1, bm1 = small["m1"]
                    m1v = m1[:].rearrange("p s g -> p (s g)")
                    S.op("dve", lambda e: e.tensor_reduce(out=m1v, in_=sel4, axis=AX.X, op=ALU.max), [bsel], [bm1])
                    eq, beq = rt["eq"]
                    eq4 = eq[:].rearrange("p s (g j) -> p (s g) j", j=4)
                    S.op("dve", lambda e: e.tensor_tensor(out=eq4, in0=sel4, in1=m1v.unsqueeze(2).broadcast_to([128, 32, 4]), op=ALU.is_ge), [bsel, bm1], [beq])
                    S.op("dve", lambda e: e.scalar_tensor_tensor(out=eq4, in0=eq4, scalar=-1e9, in1=sel4, op0=ALU.mult, op1=ALU.add), [beq, bsel], [beq])
                    m2, bm2 = small["m2"]
                    m2v = m2[:].rearrange("p s g -> p (s g)")
                    S.op("dve", lambda e: e.tensor_reduce(out=m2v, in_=eq4, axis=AX.X, op=ALU.max), [beq], [bm2])
                    gs, bgs = small["gs"]
                    S.op("dve", lambda e: e.tensor_tensor(out=gs[:], in0=m1[:], in1=m2[:], op=ALU.add), [bm1, bm2], [bgs])
                    gmax, bgmax = small1["gmax"]
                    S.op("dve", lambda e: e.tensor_reduce(out=gmax[:], in_=gs[:], axis=AX.X, op=ALU.max), [bgs], [bgmax])
                    gsel, bgsel = small["gsel"]
                    S.op("dve", lambda e: e.tensor_tensor(out=gsel[:], in0=gs[:], in1=gmax[:].unsqueeze(2).broadcast_to([128, 8, 4]), op=ALU.is_ge), [bgs, bgmax], [bgsel])
                    msk, bmsk = rt["msk"]
                    msk4 = msk[:].rearrange("p s (g j) -> p (s g) j", j=4)
                    S.op("dve", lambda e: e.tensor_tensor(out=msk4, in0=sel4, in1=m2v.unsqueeze(2).broadcast_to([128, 32, 4]), op=ALU.is_ge), [bsel, bm2], [bmsk])
                    S.op("dve", lambda e: e.tensor_tensor(out=msk4, in0=msk4, in1=gsel[:].rearrange("p s g -> p (s g)").unsqueeze(2).broadcast_to([128, 32, 4]), op=ALU.mult),
                         [bmsk, bgsel], [bmsk])
                    gate, bgate = rt["gate"]
                    S.op("dve", lambda e: e.tensor_tensor(out=gate[:], in0=msk[:], in1=sc_all[:], op=ALU.mult), [bmsk, B_sc], [bgate])
                    den, bden = small1["den"]
                    S.op("dve", lambda e: e.tensor_reduce(out=den[:], in_=gate[:], axis=AX.X, op=ALU.add), [bgate], [bden])
                    S.op("dve", lambda e: e.reciprocal(out=den[:], in_=den[:]), [bden], [bden])
                    comb, bcomb = rt["comb"]
                    S.op("dve", lambda e: e.tensor_tensor(out=comb[:], in0=gate[:], in1=den[:].unsqueeze(2).broadcast_to([128, 8, 16]), op=ALU.mult), [bgate, bden], [bcomb])
                    for s8 in range(8):
                        pc, bpc = pms.next()
                        S.op("pe", lambda e, s8=s8: e.transpose(pc[0:16, 0:128], comb[:, s8, :], identf), [bcomb, B_cst], [bpc])
                        copy("act", combT[:, s8 * 128:(s8 + 1) * 128], pc[0:16, 0:128], [bpc], [B_combT])
                    if dbg and "d_comb" in dbg_d and l == 0 and tt == 0:
                        S.dma("sp", dbg_d["d_comb"], comb[:].rearrange("p a b -> p (a b)"), reads=[bcomb], writes=[B_dbg["d_comb"]])
                    while pending:
                        resid_prev(pending.pop(0))
                    castn = {"i": 0}

                    def load_expert(x_):
                        wg_, bwg = wgu.next()
                        wd_, bwd = wdn.next()
                        pieces = []
                        for half in range(2):
                            pieces.append((wg_[:, 0, half * 4:(half + 1) * 4, :], bwg, mg_d[l, x_, half * 512:(half + 1) * 512, :].rearrange("(c p) e -> p c e", p=128), [128, 4, 512]))
                        for half in range(2):
                            pieces.append((wg_[:, 1, half * 4:(half + 1) * 4, :], bwg, mu_d[l, x_, half * 512:(half + 1) * 512, :].rearrange("(c p) e -> p c e", p=128), [128, 4, 512]))
                        for half in range(2):
                            pieces.append((wd_[:, half * 2:(half + 1) * 2, :], bwd, md_d[l, x_, half * 256:(half + 1) * 256, :].rearrange("(c p) e -> p c e", p=128), [128, 2, 1024]))
                        for dst, bdst, src, shape in pieces:
                            st, bst = stg.next()
                            stv = st[:, 0:shape[1] * shape[2]].rearrange("p (a b) -> p a b", a=shape[1])
                            S.dma("sp", stv, src, writes=[bst])
                            castn["i"] += 1
                            if castn["i"] % 2 == 0:
                                S.op("pool", lambda e, dst=dst, stv=stv: e.tensor_copy(out=dst, in_=stv), [bst], [bdst])
                            else:
                                S.op("act", lambda e, dst=dst, stv=stv: e.activation(out=dst, in_=stv, func=AF.Copy), [bst], [bdst])
                        return wg_, bwg, wd_, bwd

                    def expert_sub(x_, sub, wg_, bwg, wd_, bwd):
                        ssl = slice(sub * NX, (sub + 1) * NX)
                        pc, bpc = pgu.next()
                        S.op("pe", lambda e: e.matmul(pc[:], lhsT=selT[x_], rhs=combT[:, ssl], start=True, stop=True),
                             [B_combT, B_sel], [bpc])
                        cb, bcb = cbs.next()
                        copy("act", cb[:], pc[:], [bpc], [bcb])
                        he, bhe = hes.next()
                        for f in range(4):
                            pg, bpg = pgu.next()
                            pu, bpu = pgu.next()
                            for dc in range(8):
                                S.op("pe", lambda e, dc=dc, f=f: e.matmul(pg[:], lhsT=wg_[:, 0, dc, f * 128:(f + 1) * 128], rhs=xm[:, dc, ssl], start=(dc == 0), stop=(dc == 7)),
                                     [bwg, B_xm], [bpg], pe_accum=True)
                            for dc in range(8):
                                S.op("pe", lambda e, dc=dc, f=f: e.matmul(pu[:], lhsT=wg_[:, 1, dc, f * 128:(f + 1) * 128], rhs=xm[:, dc, ssl], start=(dc == 0), stop=(dc == 7)),
                                     [bwg, B_xm], [bpu], pe_accum=True)
                            sg, bsg = sgs.next()
                            S.op("act", lambda e: e.activation(out=sg[:], in_=pg[:], func=AF.Silu), [bpg], [bsg])
                            t1, bt1 = t1s.next()
                            S.op("dve", lambda e: e.tensor_tensor(out=t1[:], in0=pu[:], in1=sg[:], op=ALU.mult), [bpu, bsg], [bt1])
                            S.op("dve", lambda e, f=f: e.tensor_tensor(out=he[:, f, :], in0=t1[:], in1=cb[:], op=ALU.mult), [bt1, bcb], [bhe])
                        yield
                        for eo in range(8):
                            pd, bpd = pdn.next()
                            for f in range(4):
                                S.op("pe", lambda e, f=f, eo=eo: e.matmul(pd[:], lhsT=wd_[:, f, eo * 128:(eo + 1) * 128], rhs=he[:, f, :], start=(f == 0), stop=(f == 3)),
                                     [bwd, bhe], [bpd], pe_accum=True)
                            if x_ == 0:
                                copy("dve" if eo % 2 == 0 else "act", acc[:, eo, ssl], pd[:], [bpd], [B_acc[sub]])
                            else:
                                S.op("dve", lambda e, eo=eo: e.tensor_tensor(out=acc[:, eo, ssl], in0=pd[:], in1=acc[:, eo, ssl], op=ALU.add), [bpd, B_acc[sub]], [B_acc[sub]])
                        yield

                    _noload = False
                    nxt = load_expert(0)
                    for x_ in range(NE):
                        curw = nxt
                        if x_ + 1 < NE and not _noload:
                            nxt = load_expert(x_ + 1)
                        run_lockstep([expert_sub(x_, sub, *curw) for sub in range(NSX)])
                    def resid(tt_):
                        for sub in range(NS):
                            ssl = slice(sub * NT, (sub + 1) * NT)
                            tsl = slice(tt_ * TT + sub * NT, tt_ * TT + (sub + 1) * NT)
                            ht, bht = hin.next()
                            S.dma(dmaq.next(), ht[:], hT_d[:, :, tsl], reads=[B_hT], writes=[bht])
                            for fc in range(8):
                                S.op("dve", lambda e, fc=fc: e.scalar_tensor_tensor(out=ht[:, fc, :], in0=acc[:, fc, ssl], scalar=gt_ap(l, 1, fc), in1=ht[:, fc, :],
                                                                                                            op0=ALU.mult, op1=ALU.add), [B_acc[(sub * NT) // NX], bht, B_mod], [bht])
                            S.dma(dmaq.next(), hT_d[:, :, tsl], ht[:], reads=[bht], writes=[B_hT])
                    resid_prev = resid
                    pending.append(tt)
                while pending:
                    resid(pending.pop(0))
                S.barrier()

        selall = sbt(es, "selall", [16, NE, 128], BF16)
        B_sel = Buf()
        S.op("pool", lambda e: e.memset(selall[:], 0.0), [], [B_sel])
        for x_ in range(NE):
            S.op("pool", lambda e, x_=x_: e.tensor_copy(out=selall[:, x_, :], in_=cst[0:16, x_:x_ + 1].broadcast_to([16, 128])), [B_cst], [B_sel])
        selT = [selall[:, x_, :] for x_ in range(NE)]

        moe_phase(0)
        if stop_after == "moe0":
            return finish([B_hT])

        with contextlib.ExitStack() as ph:
            win = sbt(ph, "win", [128, 8, 3072], BF16)
            wout = sbt(ph, "wout", [128, 8, 1024], BF16)
            B_w1 = Buf()
            with contextlib.ExitStack() as ph2:
                stg = Ring([(sbt(ph2, "stgc%d" % i, [128, 8192], F32), Buf()) for i in range(2)])
                for p_ in range(3):
                    load_w(stg, win[:, :, p_ * 1024:(p_ + 1) * 1024], B_w1, win_d[:, p_ * 1024:(p_ + 1) * 1024].rearrange("(c p) e -> p c e", p=128), [128, 8, 1024], "win")
                load_w(stg, wout[:], B_w1, wout_d.rearrange("(c p) e -> p c e", p=128), [128, 8, 1024], "wout")
                S.barrier()
            hr = ring_tiles(ph, "hc", [128, 8, NT], F32, 2)
            xnr_ = ring_tiles(ph, "xc", [128, 8, NT], F32, 2)
            xbr = ring_tiles(ph, "xcb", [128, 8, NT], BF16, 2)
            zr = ring_tiles(ph, "zc", [128, 8, NT], BF16, 2)
            sqt = sbt(ph, "sqc", [128, 8, NT], BF16)
            B_sq = Buf()
            rstd = sbt(ph, "rstdc", [128, NT], F32)
            B_rstd = Buf()
            wkr = ring_tiles(ph, "wkc", [128, NT], F32, 8)
            psr = pslots2(ph, "psc", 14)
            def cv_tile(ti):
                tsl = slice(ti * NT, (ti + 1) * NT)
                ht, bht = hr.next()
                S.dma(dmaq.next(), ht[:], hT_d[:, :, tsl], reads=[B_hT], writes=[bht])
                xn_, bxn = xnr_.next()
                pn, bpn = psr.next()
                norm_mod(ht, bht, NT, xn_, bxn, lambda fc: sc_ap(1, 0, fc), lambda fc: sh_ap(1, 0, fc, 0), sqt, B_sq, rstd, B_rstd, pn, bpn)
                yield
                xb, bxb = xbr.next()
                copy("pool", xb[:], xn_[:], [bxn], [bxb])
                z, bz = zr.next()
                for fc in range(8):
                    pp = []
                    for p_ in range(3):
                        ps_, bps = psr.next()
                        for dc in range(8):
                            S.op("pe", lambda e, dc=dc, p_=p_: e.matmul(ps_[:], lhsT=win[:, dc, p_ * 1024 + fc * 128: p_ * 1024 + (fc + 1) * 128], rhs=xb[:, dc, :],
                                                                       start=(dc == 0), stop=(dc == 7)), [bxb, B_w1], [bps], pe_accum=True)
                        pp.append((ps_, bps))
                    (pbg, bpbg), (pcg, bpcg), (pxi, bpxi) = pp
                    yield
                    xi, bxi = wkr.next()
                    copy("act", xi[:], pxi[:], [bpxi], [bxi])
                    u, bu = wkr.next()
                    S.op("dve", lambda e: e.tensor_tensor(out=u[:], in0=pcg[:], in1=xi[:], op=ALU.mult), [bpcg, bxi], [bu])
                    cv_, bcv = wkr.next()
                    S.op("act", lambda e: e.activation(out=cv_[:], in_=u[:], func=AF.Copy, scale=vecs[:, 20, fc:fc + 1]), [bu, B_vecs], [bcv])
                    u3 = u[:].rearrange("p (r t) -> p r t", t=64)
                    c3 = cv_[:].rearrange("p (r t) -> p r t", t=64)
                    S.op("dve", lambda e: e.scalar_tensor_tensor(out=c3[:, :, 1:64], in0=u3[:, :, 0:63], scalar=vecs[:, 19, fc:fc + 1], in1=c3[:, :, 1:64], op0=ALU.mult, op1=ALU.add),
                         [bu, bcv, B_vecs], [bcv])
                    S.op("dve", lambda e: e.scalar_tensor_tensor(out=c3[:, :, 0:63], in0=u3[:, :, 1:64], scalar=vecs[:, 21, fc:fc + 1], in1=c3[:, :, 0:63], op0=ALU.mult, op1=ALU.add),
                         [bu, bcv, B_vecs], [bcv])
                    S.op("dve", lambda e: e.tensor_tensor(out=z[:, fc, :], in0=pbg[:], in1=cv_[:], op=ALU.mult), [bpbg, bcv], [bz])
                for fo in range(8):
                    yield
                    po, bpo = psr.next()
                    for dc in range(8):
                        S.op("pe", lambda e, dc=dc: e.matmul(po[:], lhsT=wout[:, dc, fo * 128:(fo + 1) * 128], rhs=z[:, dc, :], start=(dc == 0), stop=(dc == 7)),
                             [bz, B_w1], [bpo], pe_accum=True)
                    S.op("dve", lambda e: e.scalar_tensor_tensor(out=ht[:, fo, :], in0=po[:], scalar=gt_ap(1, 0, fo), in1=ht[:, fo, :], op0=ALU.mult, op1=ALU.add),
                         [bpo, bht, B_mod], [bht])
                S.dma(dmaq.next(), hT_d[:, :, tsl], ht[:], reads=[bht], writes=[B_hT])
            for t2 in range(0, T // NT, 2):
                run_lockstep([cv_tile(t2), cv_tile(t2 + 1)])
            S.barrier()

        moe_phase(1)

        with contextlib.ExitStack() as ph:
            hr = ring_tiles(ph, "hf", [128, 8, NT], F32, 2)
            xnr_ = ring_tiles(ph, "xf", [128, 8, NT], F32, 2)
            otr = ring_tiles(ph, "of", [128, 1024], F32, 2)
            sqt = sbt(ph, "sqf", [128, 8, NT], BF16)
            B_sq = Buf()
            rstd = sbt(ph, "rstdf", [128, NT], F32)
            B_rstd = Buf()
            pnr = pslots2(ph, "pnf", 2)
            pxr = Ring([(pst(ph, "pxf%d" % i, [128, 4, 128], F32), PBuf()) for i in range(4)])
            def fin_tile(ti):
                tsl = slice(ti * NT, (ti + 1) * NT)
                ht, bht = hr.next()
                S.dma(dmaq.next(), ht[:], hT_d[:, :, tsl], reads=[B_hT], writes=[bht])
                xn_, bxn = xnr_.next()
                pn, bpn = pnr.next()
                norm_mod(ht, bht, NT, xn_, bxn, lambda fc: vecs[:, 22, fc:fc + 1], None, sqt, B_sq, rstd, B_rstd, pn, bpn)
                for sub in range(2):
                    yield
                    ot, bot = otr.next()
                    for half in range(2):
                        px, bpx = pxr.next()
                        for f4 in range(4):
                            fc = half * 4 + f4
                            S.op("pe", lambda e, fc=fc, f4=f4: e.transpose(px[:, f4, :], xn_[:, fc, sub * 128:(sub + 1) * 128], identf), [bxn, B_cst], [bpx], pe_accum=True)
                        copy("dve" if half == 0 else "act", ot[:, half * 512:(half + 1) * 512], px[:].rearrange("p a b -> p (a b)"), [bpx], [bot])
                    r0 = ti * NT + sub * 128
                    S.dma(dmaq.next(), out_d[r0:r0 + 128, :], ot[:], reads=[bot], writes=[B_out])
            for t2 in range(0, T // NT, 2):
                run_lockstep([fin_tile(t2), fin_tile(t2 + 1)])
            S.final_wait("sp", [B_out] + list(B_dbg.values()))
            S.final_wait("pool", [B_out])
    return nc


def _consts():
    c = np.zeros((128, 1152), np.float32)
    i = np.arange(128)
    c[:, 0:128] = np.eye(128)
    c[:, 128:256] = (i[:, None] < i[None, :])
    c[:, 256:384] = (i[:, None] <= i[None, :])
    c[:, 384:512] = (i[:, None] > i[None, :])
    c[:, 512:640] = (i[:, None] >= i[None, :])
    c[:, 640:768] = ((i[:, None] // 64) == (i[None, :] // 64))
    c[:, 768:896] = 1.0
    m = np.ones(256, np.float32)
    m[0::128] = 0.0
    c[:, 896:1152] = m[None, :]
    return c


def _pvec(v):
    return np.ascontiguousarray(np.asarray(v, np.float32).reshape(8, 128).T)


def prep_inputs(inp):
    f = lambda a: np.ascontiguousarray(np.asarray(a, np.float32))
    vec_list = [inp["norm_g"][0, 0], inp["norm_g"][0, 1], inp["norm_g"][1, 0], inp["norm_g"][1, 1]]
    vec_list += [inp["rw_mu"][0, i] for i in range(6)]
    vec_list += [inp["rw_w0"][0, 0], inp["rw_w0"][0, 1], inp["rw_a0"][0, 0], inp["rw_a0"][0, 1]]
    vec_list += [inp["rw_k_k"][0], inp["rw_k_a"][0], np.asarray(inp["rw_r_k"][0]).reshape(-1), inp["rw_gn_w"][0], inp["rw_gn_b"][0]]
    vec_list += [inp["sc_conv"][0, i] for i in range(3)]
    vec_list += [inp["final_g"]]
    assert len(vec_list) == NV
    vecs = np.stack([_pvec(v) for v in vec_list], axis=1).reshape(128, NV * 8)
    adab = np.asarray(inp["ada_b"], np.float32).reshape(2, 48, 128).transpose(2, 0, 1).reshape(128, 96)
    shared = {
        "ada_w": f(inp["ada_w"]), "adab": f(adab), "vecs": f(vecs), "cst": _consts(),
        "rtb": f(np.broadcast_to(np.asarray(inp["router_b"], np.float32)[None, :], (128, 16))),
        "w_rkv": f(inp["rw_w_rkv"][0]), "w1": f(inp["rw_w1"][0]), "w2": f(inp["rw_w2"][0]),
        "a1": f(inp["rw_a1"][0]), "a2": f(inp["rw_a2"][0]), "g1": f(inp["rw_g1"][0]), "g2": f(inp["rw_g2"][0]),
        "w_o": f(inp["rw_w_o"][0]), "w_in": f(inp["sc_w_in"][0]), "w_out": f(inp["sc_w_out"][0]),
        "router_w": f(inp["router_w"]), "moe_g": f(inp["moe_w_gate"]), "moe_u": f(inp["moe_w_up"]), "moe_d": f(inp["moe_w_down"]),
    }
    maps = []
    cc = _pvec(inp["c_ctx"])
    for b in range(NCORE):
        m = dict(shared)
        m["x"] = f(inp["x"][b])
        m["ctx"] = f(inp["ctx"][b])
        m["cvec"] = f(np.stack([_pvec(inp["c"][b]), cc], axis=2).reshape(128, 16))
        maps.append(m)
    return maps


_NC_CACHE = {}


def kernel(**inputs):
    maps = prep_inputs(inputs)
    if "nc" not in _NC_CACHE:
        _NC_CACHE["nc"] = build_program()
    res = run_bass_kernel_spmd(_NC_CACHE["nc"], maps, core_ids=list(range(NCORE)))
    return np.stack([np.asarray(r["out"], np.float32) for r in res.results], axis=0)
```

```python
import contextlib
import numpy as np
import concourse.bass as bass
import concourse.mybir as mybir
from concourse.bass_utils import run_bass_kernel_spmd

F32 = mybir.dt.float32
BF16 = mybir.dt.bfloat16
ALU = mybir.AluOpType
AF = mybir.ActivationFunctionType
AX = mybir.AxisListType

NCORE = 8
T = 4096
D = 1024
FC = 8
CTXL = 256
NCH = 34
NE = 16
EPS = 1e-6
GN_EPS = 64e-5
DECAY_C = float(np.exp(-0.5))
NV = 23


class Buf:
    __slots__ = ("w", "r", "ds", "dram", "excl")

    def __init__(self, dram=False, excl=False):
        self.w = {}
        self.r = {}
        self.ds = None
        self.dram = dram
        self.excl = excl


def PBuf():
    return Buf(excl=True)


class _Unused:
    pass


class DSem:
    __slots__ = ("sem", "cnt")

    def __init__(self, sem):
        self.sem = sem
        self.cnt = 0


class Sched:
    ENG = ("pe", "dve", "act", "pool", "sp")

    def __init__(self, nc, es, n_dma_sems=88):
        self.nc = nc
        self.eng = {"pe": nc.tensor, "dve": nc.vector, "act": nc.scalar, "pool": nc.gpsimd, "sp": nc.sync}
        self.sem = {k: es.enter_context(nc.semaphore("s_" + k)) for k in self.ENG}
        self.cnt = {k: 0 for k in self.ENG}
        self.waited = {k: {} for k in self.ENG}
        self.engsem = set(id(s) for s in self.sem.values())
        self.free_ds = [DSem(es.enter_context(nc.semaphore("d%d" % i))) for i in range(n_dma_sems)]
        self.all_ds = list(self.free_ds)
        self.n_ops = 0
        self.bar_done = {}
        self.free_q = {k: [] for k in self.ENG}
        self.ds_bufs = []

    def get_ds(self, q):
        if self.free_q[q]:
            return self.free_q[q].pop()
        return self.free_ds.pop()

    def release(self, bufs):
        for b in bufs:
            if b.ds is not None:
                for q, d in b.ds.items():
                    self.free_q[q].append(d)
                b.ds = None

    def _waits(self, e, reads, writes, partial=False):
        deps = {}
        for b in reads:
            for s, v in b.w.items():
                if deps.get(s, (None, 0))[1] < v:
                    deps[s] = (s, v)
        for b in writes:
            if not partial:
                for s, v in b.w.items():
                    if deps.get(s, (None, 0))[1] < v:
                        deps[s] = (s, v)
            for s, v in b.r.items():
                if deps.get(s, (None, 0))[1] < v:
                    deps[s] = (s, v)
        wd = self.waited[e]
        out = []
        for s, v in deps.values():
            if wd.get(s, 0) >= v or self.bar_done.get(s, 0) >= v:
                continue
            wd[s] = v
            out.append((s, v))
        return out

    def op(self, e, fn, reads=(), writes=(), pe_accum=False):
        ex = [b for b in reads if b.excl]
        if ex:
            writes = list(writes) + ex
            reads = [b for b in reads if not b.excl]
        waits = self._waits(e, reads, writes)
        eng = self.eng[e]
        for s, v in waits:
            if pe_accum and s is self.sem[e]:
                continue
            eng.wait_ge(s, v)
        ins = fn(eng)
        self.cnt[e] += 1
        s = self.sem[e]
        c = self.cnt[e]
        ins.then_inc(s, 1)
        for b in reads:
            if b.r.get(s, 0) < c:
                b.r[s] = c
        for b in writes:
            b.w = {s: c}
            b.r = {}
        self.n_ops += 1

    def dma(self, q, out, in_, reads=(), writes=(), sembuf=None):
        waits = self._waits(q, reads, writes, partial=True)
        eng = self.eng[q]
        for s, v in waits:
            eng.wait_ge(s, v)
        sb = sembuf
        if sb is None:
            cands = [b for b in list(writes) + list(reads) if not b.dram]
            sb = cands[0] if cands else (writes[0] if writes else reads[0])
        if sb.ds is None:
            sb.ds = {}
        if q not in sb.ds:
            sb.ds[q] = self.get_ds(q)
            if sb not in self.ds_bufs:
                self.ds_bufs.append(sb)
        dsq = sb.ds[q]
        dsq.cnt += 16
        s, c = dsq.sem, dsq.cnt
        eng.dma_start(out=out, in_=in_).then_inc(s, 16)
        for b in reads:
            if b.r.get(s, 0) < c:
                b.r[s] = c
        for b in writes:
            if b.w.get(s, 0) < c:
                b.w[s] = c
        self.n_ops += 1

    def barrier(self):
        targets = [(self.sem[k], self.cnt[k]) for k in self.ENG if self.cnt[k] > 0]
        targets += [(d.sem, d.cnt) for d in self.all_ds if d.cnt > 0]
        for e in self.ENG:
            wd = self.waited[e]
            for s, v in targets:
                if s is self.sem[e]:
                    continue
                if wd.get(s, 0) >= v:
                    continue
                wd[s] = v
                self.eng[e].wait_ge(s, v)
        for s, v in targets:
            self.bar_done[s] = v
        for b in self.ds_bufs:
            if b.ds is not None:
                for q, d in b.ds.items():
                    self.free_q[q].append(d)
                b.ds = None
        self.ds_bufs = []

    def final_wait(self, e, bufs):
        for s, v in self._waits(e, bufs, ()):
            self.eng[e].wait_ge(s, v)


def lockstep_gen(gens):
    gens = list(gens)
    while gens:
        for g in list(gens):
            try:
                next(g)
            except StopIteration:
                gens.remove(g)
        yield


def run_lockstep(gens):
    gens = list(gens)
    while gens:
        for g in list(gens):
            try:
                next(g)
            except StopIteration:
                gens.remove(g)


class Ring:
    def __init__(self, items):
        self.items = items
        self.i = 0

    def next(self):
        it = self.items[self.i % len(self.items)]
        self.i += 1
        return it


def build_program(dbg=None, stop_after=None):
    nc = bass.Bass("TRN2", target_bir_lowering=False)

    def din(name, shape, dt=F32):
        return nc.dram_tensor(name, list(shape), dt, kind="ExternalInput").ap()

    def dscr(name, shape, dt=F32):
        return nc.dram_tensor(name, list(shape), dt, kind="Internal").ap()

    x_d = din("x", [T, D])
    ctx_d = din("ctx", [CTXL, D])
    cvec_d = din("cvec", [128, 16])
    adaw_d = din("ada_w", [2, D, 6 * D])
    adab_d = din("adab", [128, 96])
    vecs_d = din("vecs", [128, NV * 8])
    cst_d = din("cst", [128, 1152])
    rtb_d = din("rtb", [128, 16])
    wrkv_d = din("w_rkv", [3, D, D])
    w1_d = din("w1", [2, D, 64])
    w2_d = din("w2", [2, 64, D])
    a1_d = din("a1", [2, D, 64])
    a2_d = din("a2", [2, 64, D])
    g1_d = din("g1", [D, 128])
    g2_d = din("g2", [128, D])
    wo_d = din("w_o", [D, D])
    win_d = din("w_in", [D, 3 * D])
    wout_d = din("w_out", [D, D])
    rw_d = din("router_w", [D, NE])
    mg_d = din("moe_g", [2, NE, D, 512])
    mu_d = din("moe_u", [2, NE, D, 512])
    md_d = din("moe_d", [2, NE, 512, D])
    out_d = nc.dram_tensor("out", [T, D], F32, kind="ExternalOutput").ap()
    dbg_d = {}
    if dbg:
        for name, shape in dbg.items():
            dbg_d[name] = nc.dram_tensor(name, list(shape), F32, kind="ExternalOutput").ap()

    hT_d = dscr("hT_s", [128, FC, T])
    blk_d = dscr("blk_s", [2, NCH, 128, FC * 6 * 128], BF16)
    vtm_d = dscr("vtm_s", [NCH, 128, D], BF16)
    gT_d = dscr("gT_s", [128, FC, T], BF16)
    bon_d = dscr("bon_s", [128, FC, T])
    yT_d = dscr("yT_s", [2, 128, FC, T])

    B_hT, B_blk, B_vtm, B_gT, B_bon, B_yT, B_out = [Buf(dram=True) for _ in range(7)]
    B_dbg = {k: Buf(dram=True) for k in dbg_d}

    es = contextlib.ExitStack()
    with es:
        S = Sched(nc, es)

        uid = {"n": 0}

        def sbt(stack, name, shape, dt):
            uid["n"] += 1
            return stack.enter_context(nc.sbuf_tensor("t%d_%s" % (uid["n"], name), list(shape), dt))

        def pst(stack, name, shape, dt):
            uid["n"] += 1
            return stack.enter_context(nc.psum_tensor("p%d_%s" % (uid["n"], name), list(shape), dt))

        def pslots2(stack, name, n, width=256):
            out = []
            per = 512 // width
            for i in range((n + per - 1) // per):
                t_ = pst(stack, "%s%d" % (name, i), [128, per, width], F32)
                bb_ = PBuf()
                for j in range(per):
                    if len(out) < n:
                        out.append((t_[:, j, :], bb_))
            return Ring(out)

        dmaq = Ring(["sp"])

        cst = sbt(es, "cst", [128, 1152], F32)
        B_cst = Buf()
        cstb = sbt(es, "cstb", [128, 384], BF16)
        B_cstb = Buf()
        vecs = sbt(es, "vecs", [128, NV, 8], F32)
        B_vecs = Buf()
        mod = sbt(es, "mod", [128, 2, 48, 2], F32)
        B_mod = Buf()
        der = sbt(es, "der", [128, 5, 8], F32)
        B_der = Buf()
        rtb = sbt(es, "rtb", [128, NE], F32)
        B_rtb = Buf()
        rwt = sbt(es, "rwt", [128, FC, NE], F32)
        B_rwt = Buf()

        S.dma("sp", cst[:], cst_d, writes=[B_cst])
        S.dma("sp", vecs[:].rearrange("p v c -> p (v c)"), vecs_d, writes=[B_vecs])
        S.dma("sp", rtb[:], rtb_d, writes=[B_rtb])
        S.dma("sp", rwt[:], rw_d.rearrange("(c p) e -> p c e", p=128), writes=[B_rwt])
        identf = cst[:, 0:128]
        ident_b = cstb[:, 0:128]
        blk64_b = cstb[:, 128:256]
        ones_b = cstb[:, 256:384]
        S.op("dve", lambda e: e.tensor_copy(out=cstb[:, 0:128], in_=cst[:, 0:128]), [B_cst], [B_cstb])
        S.op("dve", lambda e: e.tensor_copy(out=cstb[:, 128:384], in_=cst[:, 640:896]), [B_cst], [B_cstb])
        MB = [cst[:, 128:384], cst[:, 384:640]]
        MT = [cst[:, 384:512], cst[:, 128:256]]
        scanmask = cst[:, 896:1152]

        def vec(i):
            return vecs[:, i, :]

        epsc = sbt(es, "epsc", [128, 2], F32)
        B_epsc = Buf()
        S.op("pool", lambda e: e.memset(epsc[:, 0:1], EPS), [], [B_epsc])
        S.op("pool", lambda e: e.memset(epsc[:, 1:2], GN_EPS), [], [B_epsc])
        omk = sbt(es, "omk", [128, 8], F32)
        B_omk = Buf()
        S.op("dve", lambda e: e.tensor_scalar(out=omk[:], in0=vecs[:, 15, :], scalar1=-1.0, scalar2=1.0, op0=ALU.mult, op1=ALU.add), [B_vecs], [B_omk])

        rr = {"i": 0}

        def ew_eng(psum=False, allow=("dve", "act", "pool")):
            cands = [a for a in allow if not (psum and a == "pool")]
            rr["i"] += 1
            return cands[rr["i"] % len(cands)]

        def copy(e, out, in_, r, w):
            if e == "act":
                S.op("act", lambda g: g.activation(out=out, in_=in_, func=AF.Copy), r, w)
            else:
                S.op(e, lambda g: g.tensor_copy(out=out, in_=in_), r, w)

        def load_w(stack_stage, dst, dst_buf, src_ap, shape, name):
            st, bst = stack_stage.next()
            n = int(np.prod(shape[1:]))
            stv = st[:, 0:n]
            if len(shape) == 3:
                stv = stv.rearrange("p (a b) -> p a b", a=shape[1])
            S.dma(dmaq.next(), stv[0:shape[0]], src_ap, writes=[bst])
            copy(ew_eng(False, ("pool", "dve", "act")), dst, stv[0:shape[0]], [bst], [dst_buf])

        with contextlib.ExitStack() as ph:
            cv = sbt(ph, "cv", [128, 8, 2], F32)
            B_cv = Buf()
            sv = sbt(ph, "sv", [128, 8, 2], F32)
            B_sv = Buf()
            adab = sbt(ph, "adab", [128, 2, 48], F32)
            B_adab = Buf()
            S.dma("sp", cv[:].rearrange("p a b -> p (a b)"), cvec_d, writes=[B_cv])
            S.dma("sp", adab[:].rearrange("p a b -> p (a b)"), adab_d, writes=[B_adab])
            S.op("act", lambda e: e.activation(out=sv[:], in_=cv[:], func=AF.Silu), [B_cv], [B_sv])
            slabs = Ring([(sbt(ph, "slab%d" % i, [128, 8, 1024], F32), Buf()) for i in range(2)])
            pm = pst(ph, "pm", [128, 8, 2], F32)
            B_pm = PBuf()
            for l in range(2):
                for sl in range(6):
                    slab, bsl = slabs.next()
                    src = adaw_d[l, :, sl * 1024:(sl + 1) * 1024].rearrange("(c p) e -> p c e", p=128)
                    S.dma("sp", slab[:, 0:4, :], src[:, 0:4, :], writes=[bsl])
                    S.dma("act", slab[:, 4:8, :], src[:, 4:8, :], writes=[bsl])
                    for ec in range(8):
                        for dc in range(8):
                            S.op("pe", lambda e, ec=ec, dc=dc, slab=slab: e.matmul(
                                pm[:, ec, :], lhsT=slab[:, dc, ec * 128:(ec + 1) * 128], rhs=sv[:, dc, :],
                                start=(dc == 0), stop=(dc == 7)),
                                [bsl, B_sv], [B_pm], pe_accum=True)
                    for j in range(2):
                        S.op("dve", lambda e, l=l, sl=sl, j=j: e.tensor_tensor(
                            out=mod[:, l, sl * 8:(sl + 1) * 8, j], in0=pm[:, :, j],
                            in1=adab[:, l, sl * 8:(sl + 1) * 8], op=ALU.add),
                            [B_pm, B_adab], [B_mod])
            for l in range(2):
                for s_ in range(2):
                    S.op("dve", lambda e, l=l, s_=s_: e.scalar_tensor_tensor(
                        out=der[:, l * 2 + s_, :], in0=mod[:, l, (1 + 3 * s_) * 8:(2 + 3 * s_) * 8, 0], scalar=1.0,
                        in1=vec(l * 2 + s_), op0=ALU.add, op1=ALU.mult), [B_mod, B_vecs], [B_der])
            S.op("dve", lambda e: e.scalar_tensor_tensor(
                out=der[:, 4, :], in0=mod[:, 0, 8:16, 1], scalar=1.0, in1=vec(0), op0=ALU.add, op1=ALU.mult),
                [B_mod, B_vecs], [B_der])
            if dbg and "d_mod" in dbg_d:
                S.dma("sp", dbg_d["d_mod"], mod[:].rearrange("p a b c -> p (a b c)"), reads=[B_mod], writes=[B_dbg["d_mod"]])
            S.barrier()

        def finish(bufs):
            if "d_hT" in dbg_d:
                S.dma("sp", dbg_d["d_hT"], hT_d.rearrange("p c t -> p (c t)"), reads=[B_hT], writes=[B_dbg["d_hT"]])
            S.final_wait("sp", list(bufs) + list(B_dbg.values()))
            return nc

        if stop_after == "prologue":
            return finish([])

        def sc_ap(l, s_, fc):
            return der[:, l * 2 + s_, fc:fc + 1]

        def sh_ap(l, s_, fc, j=0):
            return mod[:, l, 3 * s_ * 8 + fc, j:j + 1]

        def gt_ap(l, s_, fc):
            return mod[:, l, (2 + 3 * s_) * 8 + fc, 0:1]

        def norm_mod(hx, b_hx, nt, xn, b_xn, scale_fn, shift_fn, sqt, b_sq, rstd, b_rstd, pn, b_pn):
            S.op("act", lambda e: e.activation(out=sqt[:, :, 0:nt], in_=hx[:, :, 0:nt], func=AF.Square),
                 [b_hx], [b_sq])
            for fc in range(8):
                S.op("pe", lambda e, fc=fc: e.matmul(pn[:, 0:nt], lhsT=ones_b, rhs=sqt[:, fc, 0:nt],
                                                      start=(fc == 0), stop=(fc == 7)),
                     [b_sq, B_cstb], [b_pn], pe_accum=True)
            S.op("act", lambda e: e.activation(out=rstd[:, 0:nt], in_=pn[:, 0:nt], func=AF.Sqrt, bias=epsc[:, 0:1], scale=1.0 / D),
                 [b_pn, B_epsc], [b_rstd])
            S.op("dve", lambda e: e.reciprocal(out=rstd[:, 0:nt], in_=rstd[:, 0:nt]), [b_rstd], [b_rstd])
            for fc in range(8):
                if fc % 2 == 0:
                    S.op("dve", lambda e, fc=fc: e.scalar_tensor_tensor(
                        out=xn[:, fc, 0:nt], in0=hx[:, fc, 0:nt], scalar=scale_fn(fc), in1=rstd[:, 0:nt],
                        op0=ALU.mult, op1=ALU.mult), [b_hx, b_rstd, B_der, B_mod, B_vecs], [b_xn])
                else:
                    S.op("act", lambda e, fc=fc: e.activation(
                        out=xn[:, fc, 0:nt], in_=hx[:, fc, 0:nt], func=AF.Copy, scale=scale_fn(fc)),
                        [b_hx, B_der, B_mod, B_vecs], [b_xn])
                    S.op("pool", lambda e, fc=fc: e.tensor_tensor(
                        out=xn[:, fc, 0:nt], in0=xn[:, fc, 0:nt], in1=rstd[:, 0:nt], op=ALU.mult), [b_xn, b_rstd], [b_xn])
                if shift_fn is not None:
                    S.op("act", lambda e, fc=fc: e.activation(out=xn[:, fc, 0:nt], in_=xn[:, fc, 0:nt],
                                                                func=AF.Identity, bias=shift_fn(fc), scale=1.0),
                         [b_xn, B_mod], [b_xn])

        NT = 256
        gam = sbt(es, "gam", [128, 2, NCH, 8], F32)
        B_gam = Buf()
        with contextlib.ExitStack() as ph:
            wrkv = sbt(ph, "wrkv", [128, 3, 8, 1024], BF16)
            w1 = sbt(ph, "w1", [128, 2, 8, 64], BF16)
            w2 = sbt(ph, "w2", [64, 2, 1024], BF16)
            a1 = sbt(ph, "a1", [128, 2, 8, 64], BF16)
            a2 = sbt(ph, "a2", [64, 2, 1024], BF16)
            g1 = sbt(ph, "g1", [128, 8, 128], BF16)
            g2 = sbt(ph, "g2", [128, 1024], BF16)
            B_w = Buf()
            with contextlib.ExitStack() as ph2:
                stg = Ring([(sbt(ph2, "stg%d" % i, [128, 8192], F32), Buf()) for i in range(2)])
                for p_ in range(3):
                    load_w(stg, wrkv[:, p_, :, :], B_w, wrkv_d[p_].rearrange("(c p) e -> p c e", p=128), [128, 8, 1024], "wrkv")
                for d_ in range(2):
                    load_w(stg, w1[:, d_, :, :], B_w, w1_d[d_].rearrange("(c p) e -> p c e", p=128), [128, 8, 64], "w1")
                    load_w(stg, a1[:, d_, :, :], B_w, a1_d[d_].rearrange("(c p) e -> p c e", p=128), [128, 8, 64], "a1")
                    load_w(stg, w2[:, d_, :], B_w, w2_d[d_], [64, 1024], "w2")
                    load_w(stg, a2[:, d_, :], B_w, a2_d[d_], [64, 1024], "a2")
                load_w(stg, g1[:], B_w, g1_d.rearrange("(c p) e -> p c e", p=128), [128, 8, 128], "g1")
                load_w(stg, g2[:], B_w, g2_d, [128, 1024], "g2")
                S.barrier()

            if stop_after == "p1w":
                return finish([])
            xtm = Ring([(sbt(ph, "xtm%d" % i, [128, 1024], F32), Buf()) for i in range(2)])
            hx = sbt(ph, "hx", [128, 8, NT], F32)
            B_hx = Buf()
            xn = sbt(ph, "xn", [128, 8, NT], F32)
            B_xn = Buf()
            xx = sbt(ph, "xx", [128, 8, NT], F32)
            B_xx = Buf()
            sqt = sbt(ph, "sqt", [128, 8, NT], BF16)
            B_sq = Buf()
            rstd = sbt(ph, "rstd", [128, NT], F32)
            B_rstd = Buf()
            mix = [(sbt(ph, "mix%d" % i, [128, 8, NT], BF16), Buf()) for i in range(3)]
            lora = {k: (sbt(ph, "lo_" + k, [128, NT], BF16), Buf()) for k in ("w0", "w1", "a0", "a1", "g")}
            NW = 36
            wk_items = [(sbt(ph, "wk%d" % i, [128, NT], F32), Buf()) for i in range(NW)]
            wk = Ring(wk_items)
            wkb = Ring([(sbt(ph, "wkb%d" % i, [128, NT], BF16), Buf()) for i in range(14)])
            oblk = Ring([(sbt(ph, "oblk%d" % i, [128, 2, 2, 6, 128], BF16), Buf()) for i in range(2)])
            obuf2 = {id(b): Buf() for _, b in oblk.items}
            ovt = Ring([(sbt(ph, "ovt%d" % i, [128, 2, 128], BF16), Buf()) for i in range(2)])
            ogt = Ring([(sbt(ph, "ogt%d" % i, [128, NT], BF16), Buf()) for i in range(2)])
            obn = Ring([(sbt(ph, "obn%d" % i, [128, NT], F32), Buf()) for i in range(2)])
            pXr = Ring([(pst(ph, "pX%d" % i, [128, 4, 128], F32), PBuf()) for i in range(1)])
            pslots = pslots2(ph, "pp", 10)
            ptr_l = []
            for i_ in range(2):
                ptr_t = pst(ph, "ptr%d" % i_, [128, 4, 2, 128], BF16)
                bb_ = PBuf()
                ptr_l += [(ptr_t[:, i, :, :], bb_) for i in range(4)]
            ptr = Ring(ptr_l)

            for ti in range(17 if stop_after not in ("p1t0", "p1t1") else (1 if stop_after == "p1t0" else 2)):
                is_ctx = (ti == 0)
                wk = Ring(wk_items)
                src = ctx_d if is_ctx else x_d[(ti - 1) * NT: ti * NT, :]
                c0 = 0 if is_ctx else 2 + (ti - 1) * 2
                tok0 = 0 if is_ctx else (ti - 1) * NT
                rowlen = 256 if is_ctx else 64
                nrow = NT // rowlen
                for sub in range(2):
                    xt, bxt = xtm.next()
                    S.dma(dmaq.next(), xt[:], src[sub * 128:(sub + 1) * 128, :], writes=[bxt])
                    for half in range(2):
                        pX, B_pX = pXr.next()
                        for f4 in range(4):
                            fc = half * 4 + f4
                            S.op("pe", lambda e, fc=fc, f4=f4, xt=xt, pX=pX: e.transpose(pX[:, f4, :], xt[:, fc * 128:(fc + 1) * 128], identf),
                                 [bxt, B_cst], [B_pX], pe_accum=True)
                        copy("act" if half == 0 else "dve", hx[:, half * 4:(half + 1) * 4, sub * 128:(sub + 1) * 128], pX[:], [B_pX], [B_hx])
                if stop_after == "p1a":
                    return finish([])
                if not is_ctx:
                    S.dma("sp", hT_d[:, :, tok0:tok0 + NT], hx[:], reads=[B_hx], writes=[B_hT])
                pn, bpn = pslots.next()
                if is_ctx:
                    norm_mod(hx, B_hx, NT, xn, B_xn, lambda fc: der[:, 4, fc:fc + 1], lambda fc: sh_ap(0, 0, fc, 1),
                             sqt, B_sq, rstd, B_rstd, pn, bpn)
                else:
                    norm_mod(hx, B_hx, NT, xn, B_xn, lambda fc: sc_ap(0, 0, fc), lambda fc: sh_ap(0, 0, fc, 0),
                             sqt, B_sq, rstd, B_rstd, pn, bpn)
                xnr = xn[:].rearrange("p c (r t) -> p (c r) t", t=rowlen)
                xxr = xx[:].rearrange("p c (r t) -> p (c r) t", t=rowlen)
                R = rowlen
                S.op("dve", lambda e: e.tensor_tensor(out=xxr[:, :, 1:R - 1], in0=xnr[:, :, 0:R - 2], in1=xnr[:, :, 2:R], op=ALU.add),
                     [B_xn], [B_xx])
                S.op("pool", lambda e: e.tensor_copy(out=xxr[:, :, 0:1], in_=xnr[:, :, 1:2]), [B_xn], [B_xx])
                S.op("pool", lambda e: e.tensor_copy(out=xxr[:, :, R - 1:R], in_=xnr[:, :, R - 2:R - 1]), [B_xn], [B_xx])
                S.op("dve", lambda e: e.scalar_tensor_tensor(out=xx[:], in0=xx[:], scalar=0.5, in1=xn[:], op0=ALU.mult, op1=ALU.subtract),
                     [B_xn, B_xx], [B_xx])

                if stop_after == "p1b":
                    return finish([])

                def make_mix(p_, slot):
                    mt, bm = mix[slot]
                    for fc in range(8):
                        if fc % 2 == 0:
                            S.op("dve", lambda e, fc=fc: e.scalar_tensor_tensor(
                                out=mt[:, fc, :], in0=xx[:, fc, :], scalar=vecs[:, 4 + p_, fc:fc + 1], in1=xn[:, fc, :],
                                op0=ALU.mult, op1=ALU.add), [B_xx, B_xn, B_vecs], [bm])
                        else:
                            tmp_, btmp = wk.next()
                            S.op("act", lambda e, fc=fc, tmp_=tmp_: e.activation(
                                out=tmp_[:], in_=xx[:, fc, :], func=AF.Copy, scale=vecs[:, 4 + p_, fc:fc + 1]),
                                [B_xx, B_vecs], [btmp])
                            S.op("pool", lambda e, fc=fc, tmp_=tmp_: e.tensor_tensor(
                                out=mt[:, fc, :], in0=tmp_[:], in1=xn[:, fc, :], op=ALU.add), [btmp, B_xn], [bm])
                    return mt, bm

                def lora_hidden(mt, bm, wt, d_, ncol, key, func):
                    ps_, bps = pslots.next()
                    for dc in range(8):
                        lw_ = wt[:, d_, dc, :] if d_ is not None else wt[:, dc, :]
                        S.op("pe", lambda e, dc=dc, lw_=lw_: e.matmul(ps_[0:ncol, :], lhsT=lw_, rhs=mt[:, dc, :],
                                                                         start=(dc == 0), stop=(dc == 7)),
                             [bm, B_w], [bps], pe_accum=True)
                    lt, bl = lora[key]
                    S.op("act", lambda e: e.activation(out=lt[0:ncol, :], in_=ps_[0:ncol, :], func=func), [bps], [bl])

                mt, bm = make_mix(3, 0)
                lora_hidden(mt, bm, w1, 0, 64, "w0", AF.Tanh)
                lora_hidden(mt, bm, w1, 1, 64, "w1", AF.Tanh)
                mt, bm = make_mix(4, 1)
                lora_hidden(mt, bm, a1, 0, 64, "a0", AF.Copy)
                lora_hidden(mt, bm, a1, 1, 64, "a1", AF.Copy)
                mt, bm = make_mix(5, 2)
                lora_hidden(mt, bm, g1, None, 128, "g", AF.Sigmoid)
                if stop_after == "p1c":
                    return finish([])
                m_r, b_mr = make_mix(0, 0)
                m_k, b_mk = make_mix(1, 1)
                m_v, b_mv = make_mix(2, 2)

                S.barrier()
                wk = Ring(wk_items + [(t_[:, i_, :], Buf()) for t_ in (hx, xx, xn) for i_ in range(8)])

                def fc_task(fc):
                    _it = 0
                    fs = slice(fc * 128, (fc + 1) * 128)


                    def proj(mt_, bm_, p_):
                        ps_, bps = pslots.next()
                        for dc in range(8):
                            S.op("pe", lambda e, dc=dc: e.matmul(ps_[:], lhsT=wrkv[:, p_, dc, fs], rhs=mt_[:, dc, :],
                                                                  start=(dc == 0), stop=(dc == 7)),
                                 [bm_, B_w], [bps], pe_accum=True)
                        return ps_, bps

                    def small_proj(wt2, d_, key, ncol):
                        ps_, bps = pslots.next()
                        lt, bl = lora[key]
                        lhs = wt2[:, d_, fs] if d_ is not None else wt2[:, fs]
                        S.op("pe", lambda e: e.matmul(ps_[:], lhsT=lhs, rhs=lt[0:ncol, :], start=True, stop=True),
                             [bl, B_w], [bps], pe_accum=True)
                        return ps_, bps

                    p_r, b_pr = proj(m_r, b_mr, 0)
                    p_k, b_pk = proj(m_k, b_mk, 1)
                    p_v, b_pv = proj(m_v, b_mv, 2)
                    if stop_after == "p2mm" and _it == 1:
                        S.final_wait("act", [b_pr, b_pk, b_pv])
                        return finish([B_vtm, B_blk])
                    r_t, b_r = wk.next()
                    k_t, b_k = wk.next()
                    v_t, b_v = wk.next()
                    copy("act", r_t[:], p_r[:], [b_pr], [b_r])
                    copy("dve", k_t[:], p_k[:], [b_pk], [b_k])
                    copy("act", v_t[:], p_v[:], [b_pv], [b_v])
                    yield
                    kk_t, b_kk = wk.next()
                    S.op("act", lambda e: e.activation(out=kk_t[:], in_=k_t[:], func=AF.Copy, scale=vecs[:, 14, fc:fc + 1]),
                         [b_k, B_vecs], [b_kk])
                    sq_, b_sq_ = wkb.next()
                    S.op("act", lambda e: e.activation(out=sq_[:], in_=kk_t[:], func=AF.Square), [b_kk], [b_sq_])
                    pn2, bpn2 = pslots.next()
                    S.op("pe", lambda e: e.matmul(pn2[:], lhsT=blk64_b, rhs=sq_[:], start=True, stop=True), [b_sq_, B_cstb], [bpn2])
                    rn_t, b_rn = wk.next()
                    S.op("act", lambda e: e.activation(out=rn_t[:], in_=pn2[:], func=AF.Sqrt), [bpn2], [b_rn])
                    S.op("dve", lambda e: e.tensor_scalar(out=rn_t[:], in0=rn_t[:], scalar1=1e-12, scalar2=None, op0=ALU.max), [b_rn], [b_rn])
                    S.op("dve", lambda e: e.reciprocal(out=rn_t[:], in_=rn_t[:]), [b_rn], [b_rn])
                    S.op("pool", lambda e: e.tensor_tensor(out=kk_t[:], in0=kk_t[:], in1=rn_t[:], op=ALU.mult), [b_kk, b_rn], [b_kk])
                    yield
                    if not is_ctx:
                        p_g, b_pg = small_proj(g2, None, "g", 128)
                        og, bog = ogt.next()
                        copy("act", og[:], p_g[:], [b_pg], [bog])
                        S.dma("sp", gT_d[:, fc, tok0:tok0 + NT], og[:], reads=[bog], writes=[B_gT])
                    yield
                    vb_, b_vb = wkb.next()
                    copy("pool", vb_[:], v_t[:], [b_v], [b_vb])
                    pt_, bpt = ptr.next()
                    for sub in range(2):
                        S.op("pe", lambda e, sub=sub: e.transpose(pt_[:, sub, :], vb_[:, sub * 128:(sub + 1) * 128], ident_b),
                             [b_vb, B_cstb], [bpt], pe_accum=True)
                    ov, bov = ovt.next()
                    copy("dve", ov[:], pt_[:], [bpt], [bov])
                    S.dma("sp", vtm_d[c0:c0 + 2, :, fs].rearrange("c p e -> p c e"), ov[:], reads=[bov], writes=[B_vtm])

                    yield
                    ob, bob0 = oblk.next()
                    bobs = [bob0, obuf2[id(bob0)]]
                    kds = [None, None]

                    def dir_task(d_):
                        bob = bobs[d_]
                        p_w, b_pw = small_proj(w2, d_, "w%d" % d_, 64)
                        p_a, b_pa = small_proj(a2, d_, "a%d" % d_, 64)
                        sg_t, b_sg = wk.next()
                        S.op("act", lambda e: e.activation(out=sg_t[:], in_=p_w[:], func=AF.Sigmoid, bias=vecs[:, 10 + d_, fc:fc + 1], scale=1.0),
                             [b_pw, B_vecs], [b_sg])
                        a_t, b_a = wk.next()
                        S.op("act", lambda e: e.activation(out=a_t[:], in_=p_a[:], func=AF.Sigmoid, bias=vecs[:, 12 + d_, fc:fc + 1], scale=1.0),
                             [b_pa, B_vecs], [b_a])
                        yield
                        lw_t, b_lw = wk.next()
                        S.op("act", lambda e: e.activation(out=lw_t[:], in_=sg_t[:], func=AF.Copy, scale=-DECAY_C),
                             [b_sg], [b_lw])
                        kka, b_kka = wk.next()
                        S.op("pool", lambda e: e.tensor_tensor(out=kka[:], in0=kk_t[:], in1=a_t[:], op=ALU.mult), [b_kk, b_a], [b_kka])
                        kd, b_kd = wk.next()
                        S.op("act", lambda e: e.activation(out=kd[:], in_=a_t[:], func=AF.Identity, scale=vecs[:, 15, fc:fc + 1], bias=omk[:, fc:fc + 1]),
                             [b_a, B_vecs, B_omk], [b_kd])
                        S.op("pool", lambda e: e.tensor_tensor(out=kd[:], in0=kd[:], in1=k_t[:], op=ALU.mult),
                             [b_kd, b_k], [b_kd])
                        kds[d_] = (kd, b_kd)
                        yield
                        L_t, b_L = wk.next()
                        S.op("dve", lambda e: e.tensor_tensor_scan(out=L_t[:], data0=scanmask, data1=lw_t[:], initial=0.0,
                                                                   op0=ALU.mult, op1=ALU.add), [b_lw, B_cst], [b_L])
                        yield
                        Lx_t, b_Lx = wk.next()
                        S.op("pool", lambda e: e.tensor_tensor(out=Lx_t[:], in0=L_t[:], in1=lw_t[:], op=ALU.subtract), [b_L, b_lw], [b_Lx])
                        D_t, b_D = wk.next()
                        L3 = L_t[:].rearrange("p (c t) -> p c t", t=128)
                        Tot = L3[:, :, 127:128]
                        S.op("dve", lambda e: e.tensor_tensor(out=D_t[:].rearrange("p (c t) -> p c t", t=128),
                                                              in0=Tot.broadcast_to([128, 2, 128]), in1=L3, op=ALU.subtract),
                             [b_L], [b_D])
                        S.op("act", lambda e: e.activation(out=gam[:, d_, c0:c0 + 2, fc], in_=L3[:, :, 127], func=AF.Exp),
                             [b_L], [B_gam])
                        yield
                        ea, b_ea = wk.next()
                        eb, b_eb = wk.next()
                        er, b_er = wk.next()
                        eh, b_eh = wk.next()
                        if d_ == 0:
                            S.op("act", lambda e: e.activation(out=ea[:], in_=Lx_t[:], func=AF.Exp), [b_Lx], [b_ea])
                            S.op("act", lambda e: e.activation(out=eb[:], in_=L_t[:], func=AF.Exp, scale=-1.0), [b_L], [b_eb])
                            S.op("act", lambda e: e.activation(out=er[:], in_=L_t[:], func=AF.Exp), [b_L], [b_er])
                            S.op("act", lambda e: e.activation(out=eh[:], in_=D_t[:], func=AF.Exp), [b_D], [b_eh])
                        else:
                            Dl, b_Dl = wk.next()
                            S.op("pool", lambda e: e.tensor_tensor(out=Dl[:], in0=D_t[:], in1=lw_t[:], op=ALU.add), [b_D, b_lw], [b_Dl])
                            S.op("act", lambda e: e.activation(out=ea[:], in_=D_t[:], func=AF.Exp), [b_D], [b_ea])
                            S.op("act", lambda e: e.activation(out=eb[:], in_=Dl[:], func=AF.Exp, scale=-1.0), [b_Dl], [b_eb])
                            S.op("act", lambda e: e.activation(out=er[:], in_=Dl[:], func=AF.Exp), [b_Dl], [b_er])
                            S.op("act", lambda e: e.activation(out=eh[:], in_=Lx_t[:], func=AF.Exp), [b_Lx], [b_eh])
                        yield

                        def o4(q):
                            return ob[:, d_, :, q, :]

                        def v3(t_):
                            return t_[:].rearrange("p (c t) -> p c t", t=128)
                        S.op("dve", lambda e: e.scalar_tensor_tensor(out=o4(0), in0=v3(kk_t), scalar=-1.0, in1=v3(ea), op0=ALU.mult, op1=ALU.mult),
                             [b_kk, b_ea], [bob])
                        S.op("pool", lambda e: e.tensor_tensor(out=o4(1), in0=v3(r_t), in1=v3(er), op=ALU.mult), [b_r, b_er], [bob])
                        S.op("dve", lambda e: e.tensor_tensor(out=o4(2), in0=v3(kka), in1=v3(eb), op=ALU.mult), [b_kka, b_eb], [bob])
                        S.op("pool", lambda e: e.tensor_tensor(out=o4(3), in0=v3(kd), in1=v3(eb), op=ALU.mult), [b_kd, b_eb], [bob])
                        yield
                        for q, srct, bsrc in ((4, kka, b_kka), (5, kd, b_kd)):
                            hb, b_hb = wkb.next()
                            S.op("dve" if q == 4 else "pool", lambda e, srct=srct, hb=hb: e.tensor_tensor(out=hb[:], in0=srct[:], in1=eh[:], op=ALU.mult),
                                 [bsrc, b_eh], [b_hb])
                            pt2, bpt2 = ptr.next()
                            for sub in range(2):
                                S.op("pe", lambda e, sub=sub, hb=hb, pt2=pt2: e.transpose(pt2[:, sub, :], hb[:, sub * 128:(sub + 1) * 128], ident_b),
                                     [b_hb, B_cstb], [bpt2], pe_accum=True)
                            copy("act" if q == 4 else "dve", ob[:, d_, :, q, :], pt2[:], [bpt2], [bob])
                            yield
                        dst = blk_d[d_, c0:c0 + 2, :, fc * 768:(fc + 1) * 768].rearrange("c p (q t) -> p c q t", q=6)
                        S.dma("sp", dst, ob[:, d_, :, :, :], reads=[bob], writes=[B_blk])

                    yield from lockstep_gen([dir_task(0), dir_task(1)])
                    if not is_ctx:
                        (kd0, b_kd0), (kd1, b_kd1) = kds
                        S.op("pool", lambda e: e.tensor_tensor(out=kd1[:], in0=kd1[:], in1=kd0[:], op=ALU.add), [b_kd1, b_kd0], [b_kd1])
                        pb_, b_pb = wkb.next()
                        S.op("dve", lambda e: e.scalar_tensor_tensor(out=pb_[:], in0=r_t[:], scalar=vecs[:, 16, fc:fc + 1], in1=kd1[:],
                                                                     op0=ALU.mult, op1=ALU.mult), [b_r, b_kd1, B_vecs], [b_pb])
                        pn3, bpn3 = pslots.next()
                        S.op("pe", lambda e: e.matmul(pn3[:], lhsT=blk64_b, rhs=pb_[:], start=True, stop=True), [b_pb, B_cstb], [bpn3])
                        obn_, bobn = obn.next()
                        S.op("dve", lambda e: e.tensor_tensor(out=obn_[:], in0=pn3[:], in1=v_t[:], op=ALU.mult), [bpn3, b_v], [bobn])
                        S.dma("sp", bon_d[:, fc, tok0:tok0 + NT], obn_[:], reads=[bobn], writes=[B_bon])

                for fa in range(0, 8, 2):
                    run_lockstep([fc_task(fa), fc_task(fa + 1)])
                S.barrier()
            S.barrier()

        if dbg and "d_gam" in dbg_d:
            S.dma("sp", dbg_d["d_gam"], gam[:].rearrange("p a b c -> p (a b c)"), reads=[B_gam], writes=[B_dbg["d_gam"]])
        if stop_after in ("phase1", "p1t0", "p1t1"):
            return finish([B_hT, B_blk, B_vtm, B_gT, B_bon])

        with contextlib.ExitStack() as ph:
            St = sbt(ph, "St", [128, 2, 8, 64], F32)
            Sb = sbt(ph, "Sb", [128, 2, 8, 64], BF16)
            B_S = [[Buf() for _ in range(4)] for _ in range(2)]
            S.op("dve", lambda e: e.memset(St[:], 0.0), [], [b for bb in B_S for b in bb])
            S.op("pool", lambda e: e.memset(Sb[:], 0.0), [], [b for bb in B_S for b in bb])
            blkin = [Ring([(sbt(ph, "bi%d_%d" % (d_, i), [128, 8, 6, 128], BF16), Buf()) for i in range(2)]) for d_ in range(2)]
            vin = [Ring([(sbt(ph, "vi%d_%d" % (d_, i), [128, 1024], BF16), Buf()) for i in range(2)]) for d_ in range(2)]
            NG = 3
            gctx = Ring([dict(
                AB=sbt(ph, "AB%d" % i, [128, 4, 256], BF16), AK=sbt(ph, "AK%d" % i, [128, 4, 256], BF16),
                A32=sbt(ph, "A32%d" % i, [128, 4, 128], F32),
                AT=sbt(ph, "AT%d" % i, [128, 4, 128], F32),
                PM=[sbt(ph, "PM%d_%d" % (i, j), [128, 4, 256], F32) for j in range(2)],
                PT=[sbt(ph, "PT%d_%d" % (i, j), [128, 4, 128], F32) for j in range(2)],
                MI=sbt(ph, "MI%d" % i, [128, 4, 128], BF16),
                W=sbt(ph, "W%d" % i, [128, 4, 64], BF16), U=sbt(ph, "U%d" % i, [128, 4, 64], BF16),
                b={k: Buf() for k in ("AB", "A32", "AK", "AT", "PM0", "PM1", "PT0", "PT1", "MI", "W", "U")})
                for i in range(NG)])
            ysb = [Ring([(sbt(ph, "ys%d_%d" % (d_, i), [128, 8, 128], F32), Buf()) for i in range(2)]) for d_ in range(2)]
            PAg = [(pst(ph, "PA%d" % i, [128, 4, 256], F32), PBuf()) for i in range(2)]
            PTg = [(pst(ph, "PTg%d" % i, [128, 4, 128], F32), PBuf()) for i in range(2)]
            PH = [pst(ph, "PH%d" % i, [128, 512], F32) for i in range(2)]
            B_PH = PBuf()
            PWv = [PH[h][:, 0:128].rearrange("p (j v) -> p j v", j=2) for h in range(2)]
            PUv = [PH[h][:, 128:256].rearrange("p (j v) -> p j v", j=2) for h in range(2)]
            PYv = PH[0][:, 256:512].rearrange("p (j t) -> p j t", j=2)
            PSv = PH[1][:, 256:384].rearrange("p (j v) -> p j v", j=2)

            order = [list(range(NCH)), [1, 0] + list(range(NCH - 1, 1, -1))]
            for step in range(NCH):
                cur = []
                for d_ in range(2):
                    c = order[d_][step]
                    bt, bb = blkin[d_].next()
                    S.dma("sp", bt[:].rearrange("p f q t -> p (f q t)"), blk_d[d_, c], reads=[B_blk], writes=[bb])
                    vt, bv = vin[d_].next()
                    S.dma("sp" if d_ == 0 else "sp", vt[:], vtm_d[c], reads=[B_vtm], writes=[bv])
                    cur.append((c, bt, bb, vt, bv))
                if stop_after == "s_0":
                    S.final_wait("act", [cur[0][2], cur[0][4], cur[1][2], cur[1][4]] + [b for bb in B_S for b in bb]); return finish([])
                ys = [None, None]
                for d_ in range(2):
                    if cur[d_][0] >= 2:
                        ys[d_] = ysb[d_].next()
                def grp(q, d_):
                    c, bt, bb, vt, bv = cur[d_]
                    G = gctx.next()
                    gb = G["b"]
                    items = [(j, hh) for hh in range(2) for j in range(2)]
                    H2 = (slice(0, 2), slice(2, 4))
                    pa, bpa = PAg[d_]
                    ptp, bptp = PTg[d_]
                    mb = MB[d_].unsqueeze(1).broadcast_to([128, 4, 256])
                    mtm = MT[d_].unsqueeze(1).broadcast_to([128, 4, 128])

                    def a_mm(q_lhs, rhs_fn, out_fn):
                        for i, (j, hh) in enumerate(items):
                            fc = 2 * q + j
                            hs = slice(hh * 64, hh * 64 + 64)
                            S.op("pe", lambda e, i=i, fc=fc, hs=hs: e.matmul(out_fn(i), lhsT=bt[hs, fc, q_lhs, :], rhs=rhs_fn(hs, fc), start=True, stop=True),
                                 [bb], [bpa], pe_accum=True)
                    a_mm(2, lambda hs, fc: bt[hs, fc, 0:2, :], lambda i: pa[:, i, :])
                    for h2 in H2:
                        S.op("dve", lambda e, h2=h2: e.tensor_tensor(out=G["AB"][:, h2, :], in0=pa[:, h2, :], in1=mb[:, h2, :], op=ALU.mult), [bpa, B_cst], [gb["AB"]])
                    for h2 in H2:
                        S.op("act", lambda e, h2=h2: e.activation(out=G["A32"][:, h2, :], in_=pa[:, h2, 0:128], func=AF.Copy), [bpa], [gb["A32"]])
                    S.op("pool", lambda e: e.tensor_tensor(out=G["A32"][:], in0=G["A32"][:], in1=mb[:, :, 0:128], op=ALU.mult), [gb["A32"], B_cst], [gb["A32"]])
                    S.op("pool", lambda e: e.tensor_tensor(out=G["PM"][1][:, :, 128:256], in0=G["A32"][:],
                                                           in1=identf.unsqueeze(1).broadcast_to([128, 4, 128]), op=ALU.add),
                         [gb["A32"], B_cst], [gb["PM1"]])
                    yield
                    a_mm(3, lambda hs, fc: bt[hs, fc, 0:2, :], lambda i: pa[:, i, :])
                    for h2 in H2:
                        S.op("dve", lambda e, h2=h2: e.tensor_tensor(out=G["AK"][:, h2, :], in0=pa[:, h2, :], in1=mb[:, h2, :], op=ALU.mult), [bpa, B_cst], [gb["AK"]])
                    yield
                    a_mm(0, lambda hs, fc: bt[hs, fc, 2, :], lambda i: pa[:, i, 0:128])
                    for h2 in H2:
                        S.op("dve", lambda e, h2=h2: e.tensor_tensor(out=G["AT"][:, h2, :], in0=pa[:, h2, 0:128], in1=mtm[:, h2, :], op=ALU.mult), [bpa, B_cst], [gb["AT"]])
                    yield
                    for i in range(4):
                        S.op("pe", lambda e, i=i: e.matmul(pa[:, i, 0:128], lhsT=G["AT"][:, i, :], rhs=G["A32"][:, i, :], start=True, stop=True),
                             [gb["AT"], gb["A32"]], [bpa], pe_accum=True)
                    for i in range(4):
                        S.op("pe", lambda e, i=i: e.matmul(ptp[:, i, :], lhsT=G["A32"][:, i, :], rhs=G["AT"][:, i, :], start=True, stop=True),
                             [gb["AT"], gb["A32"]], [bptp], pe_accum=True)
                    for h2 in H2:
                        S.op("act", lambda e, h2=h2: e.activation(out=G["PM"][1][:, h2, 0:128], in_=pa[:, h2, 0:128], func=AF.Copy), [bpa], [gb["PM1"]])
                    S.op("dve", lambda e: e.tensor_copy(out=G["PT"][1][:], in_=ptp[:]), [bptp], [gb["PT1"]])
                    yield
                    for k in range(1, 6):
                        cu, nx = k % 2, (k + 1) % 2
                        PMc, PTc, PMn, PTn = G["PM"][cu], G["PT"][cu], G["PM"][nx], G["PT"][nx]
                        bPMc, bPTc, bPMn, bPTn = gb["PM%d" % cu], gb["PT%d" % cu], gb["PM%d" % nx], gb["PT%d" % nx]
                        last = (k == 5)
                        for i in range(4):
                            rhs_ = PMc[:, i, 128:256] if last else PMc[:, i, :]
                            out_ = pa[:, i, 128:256] if last else pa[:, i, :]
                            S.op("pe", lambda e, i=i, rhs_=rhs_, out_=out_, PTc=PTc: e.matmul(out_, lhsT=PTc[:, i, :], rhs=rhs_, start=True, stop=True),
                                 [bPMc, bPTc], [bpa], pe_accum=True)
                        for i in range(4):
                            S.op("pe", lambda e, i=i, PMc=PMc, PTc=PTc: e.matmul(ptp[:, i, :], lhsT=PMc[:, i, 0:128], rhs=PTc[:, i, :], start=True, stop=True),
                                 [bPMc, bPTc], [bptp], pe_accum=True)
                        if not last:
                            for h2 in H2:
                                S.op("act", lambda e, PMn=PMn, h2=h2: e.activation(out=PMn[:, h2, 0:128], in_=pa[:, h2, 0:128], func=AF.Copy), [bpa], [bPMn])
                        for h2 in H2:
                            S.op("dve", lambda e, PMn=PMn, PMc=PMc, h2=h2: e.tensor_tensor(out=PMn[:, h2, 128:256], in0=pa[:, h2, 128:256], in1=PMc[:, h2, 128:256], op=ALU.add),
                                 [bpa, bPMc], [bPMn])
                        S.op("act" if k % 2 else "dve", (lambda e, PTn=PTn: e.activation(out=PTn[:], in_=ptp[:], func=AF.Copy)) if k % 2 else (lambda e, PTn=PTn: e.tensor_copy(out=PTn[:], in_=ptp[:])),
                             [bptp], [bPTn])
                        yield
                    PM6, PT6 = G["PM"][0], G["PT"][0]
                    for i in range(4):
                        S.op("pe", lambda e, i=i: e.matmul(pa[:, i, 128:256], lhsT=PT6[:, i, :], rhs=PM6[:, i, 128:256], start=True, stop=True),
                             [gb["PM0"], gb["PT0"]], [bpa], pe_accum=True)
                    for h2 in H2:
                        S.op("dve", lambda e, h2=h2: e.tensor_tensor(out=G["MI"][:, h2, :], in0=pa[:, h2, 128:256], in1=PM6[:, h2, 128:256], op=ALU.add),
                             [bpa, gb["PM0"]], [gb["MI"]])
                    yield
                    bS = B_S[d_][q]
                    for i, (j, hh) in enumerate(items):
                        fc = 2 * q + j
                        hs = slice(hh * 64, hh * 64 + 64)
                        S.op("pe", lambda e, i=i, fc=fc, hs=hs: e.matmul(PWv[i // 2][:, i % 2, :], lhsT=bt[hs, fc, 0, :], rhs=Sb[hs, d_, fc, :], start=True, stop=False),
                             [bb, bS], [B_PH], pe_accum=True)
                        S.op("pe", lambda e, i=i, fc=fc, hh=hh: e.matmul(PWv[i // 2][:, i % 2, :], lhsT=G["AK"][:, i, 0:128], rhs=vt[:, fc * 128 + hh * 64: fc * 128 + hh * 64 + 64],
                                                                        start=False, stop=True),
                             [gb["AK"], bv], [B_PH], pe_accum=True)
                    for h_ in range(2):
                        S.op("act", lambda e, h_=h_: e.activation(out=G["W"][:, 2 * h_:2 * h_ + 2, :], in_=PWv[h_], func=AF.Copy), [B_PH], [gb["W"]])
                    yield
                    for i in range(4):
                        S.op("pe", lambda e, i=i: e.matmul(PUv[i // 2][:, i % 2, :], lhsT=G["MI"][:, i, :], rhs=G["W"][:, i, :], start=True, stop=True),
                             [gb["MI"], gb["W"]], [B_PH], pe_accum=True)
                    for h_ in range(2):
                        S.op("act", lambda e, h_=h_: e.activation(out=G["U"][:, 2 * h_:2 * h_ + 2, :], in_=PUv[h_], func=AF.Copy), [B_PH], [gb["U"]])
                    yield
                    for i, (j, hh) in enumerate(items):
                        fc = 2 * q + j
                        hs = slice(hh * 64, hh * 64 + 64)
                        vsl = vt[:, fc * 128 + hh * 64: fc * 128 + hh * 64 + 64]
                        if c >= 2:
                            S.op("pe", lambda e, fc=fc, hs=hs, j=j: e.matmul(PYv[hs, j, :], lhsT=Sb[hs, d_, fc, :], rhs=bt[hs, fc, 1, :], start=True, stop=False),
                                 [bb, bS], [B_PH], pe_accum=True)
                            S.op("pe", lambda e, i=i, hs=hs, j=j: e.matmul(PYv[hs, j, :], lhsT=G["U"][:, i, :], rhs=G["AB"][:, i, 128:256], start=False, stop=False),
                                 [gb["U"], gb["AB"]], [B_PH], pe_accum=True)
                            S.op("pe", lambda e, i=i, hs=hs, j=j, vsl=vsl: e.matmul(PYv[hs, j, :], lhsT=vsl, rhs=G["AK"][:, i, 128:256], start=False, stop=True),
                                 [bv, gb["AK"]], [B_PH], pe_accum=True)
                        S.op("pe", lambda e, i=i, fc=fc, hs=hs, j=j: e.matmul(PSv[hs, j, :], lhsT=bt[:, fc, 4, hs], rhs=G["U"][:, i, :], start=True, stop=False),
                             [bb, gb["U"]], [B_PH], pe_accum=True)
                        S.op("pe", lambda e, fc=fc, hs=hs, j=j, vsl=vsl: e.matmul(PSv[hs, j, :], lhsT=bt[:, fc, 5, hs], rhs=vsl, start=False, stop=True),
                             [bb, bv], [B_PH], pe_accum=True)
                    if c >= 2:
                        yt_, byt = ys[d_]
                        S.op("act", lambda e: e.activation(out=yt_[:, 2 * q:2 * q + 2, :], in_=PYv, func=AF.Copy), [B_PH], [byt])
                    for j in range(2):
                        fc = 2 * q + j
                        S.op("dve", lambda e, fc=fc, j=j: e.scalar_tensor_tensor(out=St[:, d_, fc, :], in0=St[:, d_, fc, :], scalar=gam[:, d_, c, fc:fc + 1],
                                                                                 in1=PSv[:, j, :], op0=ALU.mult, op1=ALU.add),
                             [B_PH, bS, B_gam], [bS])
                    S.op("pool", lambda e: e.tensor_copy(out=Sb[:, d_, 2 * q:2 * q + 2, :], in_=St[:, d_, 2 * q:2 * q + 2, :]), [bS], [bS])
                    yield

                for q in range(4):
                    run_lockstep([grp(q, 0), grp(q, 1)])
                if stop_after == "s_g" or (stop_after == "s_h" and step == 2):
                    S.final_wait("sp", [b for bb in B_S for b in bb]); return finish([])
                for d_ in range(2):
                    c = cur[d_][0]
                    if c >= 2:
                        yt_, byt = ys[d_]
                        S.dma("sp", yT_d[d_, :, :, (c - 2) * 128:(c - 1) * 128], yt_[:], reads=[byt], writes=[B_yT])
            S.barrier()
        if stop_after == "scan":
            return finish([B_yT, B_hT])

        def ring_tiles(ph, name, shape, dt, n):
            return Ring([(sbt(ph, "%s%d" % (name, i), shape, dt), Buf()) for i in range(n)])

        with contextlib.ExitStack() as ph:
            wo = sbt(ph, "wo", [128, 8, 1024], BF16)
            B_wo = Buf()
            with contextlib.ExitStack() as ph2:
                stg = Ring([(sbt(ph2, "stgr%d" % i, [128, 8192], F32), Buf()) for i in range(1)])
                load_w(stg, wo[:], B_wo, wo_d.rearrange("(c p) e -> p c e", p=128), [128, 8, 1024], "wo")
                S.barrier()
            y0r = ring_tiles(ph, "y0r", [128, 8, NT], F32, 2)
            y1r = ring_tiles(ph, "y1r", [128, 8, NT], F32, 2)
            bnr = ring_tiles(ph, "bnr", [128, 8, NT], F32, 2)
            gtr = ring_tiles(ph, "gtr", [128, 8, NT], BF16, 2)
            hr = ring_tiles(ph, "hr", [128, 8, NT], F32, 2)
            zb = ring_tiles(ph, "zb", [128, 8, NT], BF16, 2)
            wkr = ring_tiles(ph, "wkr", [128, NT], F32, 8)
            wkbr = ring_tiles(ph, "wkbr", [128, NT], BF16, 4)
            psr = pslots2(ph, "psr", 8)
            def ro_tile(ti):
                tsl = slice(ti * NT, (ti + 1) * NT)
                y0, by0 = y0r.next()
                y1, by1 = y1r.next()
                bn, bbn = bnr.next()
                gt, bgt = gtr.next()
                ht, bht = hr.next()
                S.dma("sp", y0[:], yT_d[0, :, :, tsl], reads=[B_yT], writes=[by0])
                S.dma("sp", y1[:], yT_d[1, :, :, tsl], reads=[B_yT], writes=[by1])
                S.dma("sp", bn[:], bon_d[:, :, tsl], reads=[B_bon], writes=[bbn])
                S.dma("sp", gt[:], gT_d[:, :, tsl], reads=[B_gT], writes=[bgt])
                S.dma("sp", ht[:], hT_d[:, :, tsl], reads=[B_hT], writes=[bht])
                S.op("pool", lambda e: e.tensor_tensor(out=y0[:], in0=y0[:], in1=y1[:], op=ALU.add), [by0, by1], [by0])
                S.op("pool", lambda e: e.tensor_tensor(out=y0[:], in0=y0[:], in1=bn[:], op=ALU.add), [by0, bbn], [by0])
                z, bz = zb.next()
                yield
                for fc in range(8):
                    yb, byb = wkbr.next()
                    copy("act", yb[:], y0[:, fc, :], [by0], [byb])
                    pmn, bpmn = psr.next()
                    S.op("pe", lambda e: e.matmul(pmn[:], lhsT=blk64_b, rhs=yb[:], start=True, stop=True), [byb, B_cstb], [bpmn])
                    dd, bdd = wkr.next()
                    S.op("dve", lambda e: e.scalar_tensor_tensor(out=dd[:], in0=pmn[:], scalar=-1.0 / 64, in1=y0[:, fc, :], op0=ALU.mult, op1=ALU.add),
                         [bpmn, by0], [bdd])
                    yield
                    sqb, bsqb = wkbr.next()
                    S.op("act", lambda e: e.activation(out=sqb[:], in_=dd[:], func=AF.Square), [bdd], [bsqb])
                    pvr, bpvr = psr.next()
                    S.op("pe", lambda e: e.matmul(pvr[:], lhsT=blk64_b, rhs=sqb[:], start=True, stop=True), [bsqb, B_cstb], [bpvr])
                    yield
                    rs, brs = wkr.next()
                    S.op("act", lambda e: e.activation(out=rs[:], in_=pvr[:], func=AF.Sqrt, bias=epsc[:, 1:2], scale=1.0 / 64), [bpvr, B_epsc], [brs])
                    S.op("dve", lambda e: e.reciprocal(out=rs[:], in_=rs[:]), [brs], [brs])
                    S.op("pool", lambda e: e.tensor_tensor(out=dd[:], in0=dd[:], in1=rs[:], op=ALU.mult), [bdd, brs], [bdd])
                    S.op("act", lambda e: e.activation(out=dd[:], in_=dd[:], func=AF.Identity, bias=vecs[:, 18, fc:fc + 1], scale=vecs[:, 17, fc:fc + 1]),
                         [bdd, B_vecs], [bdd])
                    S.op("pool", lambda e: e.tensor_tensor(out=z[:, fc, :], in0=dd[:], in1=gt[:, fc, :], op=ALU.mult), [bdd, bgt], [bz])
                for fo in range(8):
                    yield
                    po, bpo = psr.next()
                    for dc in range(8):
                        S.op("pe", lambda e, dc=dc: e.matmul(po[:], lhsT=wo[:, dc, fo * 128:(fo + 1) * 128], rhs=z[:, dc, :], start=(dc == 0), stop=(dc == 7)),
                             [bz, B_wo], [bpo], pe_accum=True)
                    S.op("dve", lambda e: e.scalar_tensor_tensor(out=ht[:, fo, :], in0=po[:], scalar=gt_ap(0, 0, fo), in1=ht[:, fo, :], op0=ALU.mult, op1=ALU.add),
                         [bpo, bht, B_mod], [bht])
                S.dma("sp", hT_d[:, :, tsl], ht[:], reads=[bht], writes=[B_hT])
            for t2 in range(0, T // NT, 2):
                run_lockstep([ro_tile(t2), ro_tile(t2 + 1)])
            S.barrier()

        if stop_after == "readout":
            return finish([B_hT])

        def moe_phase(l):
            TT = 1024
            NS = TT // NT
            NX = 512
            NSX = TT // NX
            with contextlib.ExitStack() as ph:
                wgu = ring_tiles(ph, "wgu", [128, 2, 8, 512], BF16, 2)
                wdn = ring_tiles(ph, "wdn", [128, 4, 1024], BF16, 2)
                stg = ring_tiles(ph, "stgm", [128, 2048], F32, 4)
                xm = sbt(ph, "xm", [128, 8, TT], BF16)
                B_xm = Buf()
                acc = sbt(ph, "acc", [128, 8, TT], F32)
                B_acc = [Buf() for _ in range(NSX)]
                hin = ring_tiles(ph, "hin", [128, 8, NT], F32, 2)
                xm32 = ring_tiles(ph, "xm32", [128, 8, NT], F32, 1)
                sqt = sbt(ph, "sqm", [128, 8, NT], BF16)
                B_sq = Buf()
                rstd = sbt(ph, "rstdm", [128, NT], F32)
                B_rstd = Buf()
                sc_all = sbt(ph, "sc_all", [128, 8, 16], F32)
                B_sc = Buf()
                rt = {k: (sbt(ph, "rt_" + k, [128, 8, 16], F32), Buf()) for k in ("sel", "m1", "eq", "m2", "gs", "gsel", "msk", "gate", "comb")}
                small = {k: (sbt(ph, "sm_" + k, [128, 8, 4], F32), Buf()) for k in ("m1", "m2", "gs", "gsel")}
                small1 = {k: (sbt(ph, "s1_" + k, [128, 8], F32), Buf()) for k in ("gmax", "den")}
                combT = sbt(ph, "combT", [16, TT], BF16)
                B_combT = Buf()
                cbs = ring_tiles(ph, "cbs", [128, NX], F32, 2)
                sgs = ring_tiles(ph, "sgs", [128, NX], F32, 3)
                t1s = ring_tiles(ph, "t1s", [128, NX], F32, 3)
                hes = ring_tiles(ph, "hes", [128, 4, NX], BF16, 2)
                pgu = pslots2(ph, "pgu", 4, width=512)
                pdn = pslots2(ph, "pdn", 3, width=512)
                pms = pslots2(ph, "pms", 2)

                pending = []
                resid_prev = None
                for tt in range(T // TT):
                    for sub in range(NS):
                        tsl = slice(tt * TT + sub * NT, tt * TT + (sub + 1) * NT)
                        ht, bht = hin.next()
                        S.dma(dmaq.next(), ht[:], hT_d[:, :, tsl], reads=[B_hT], writes=[bht])
                        x32, bx32 = xm32.next()
                        pn, bpn = pms.next()
                        norm_mod(ht, bht, NT, x32, bx32, lambda fc: sc_ap(l, 1, fc), lambda fc: sh_ap(l, 1, fc, 0),
                                 sqt, B_sq, rstd, B_rstd, pn, bpn)
                        copy("pool", xm[:, :, sub * NT:(sub + 1) * NT], x32[:], [bx32], [B_xm])
                        pr, bpr = pms.next()
                        for s2 in range(2):
                            for dc in range(8):
                                S.op("pe", lambda e, s2=s2, dc=dc: e.matmul(pr[:, s2 * 16:(s2 + 1) * 16], lhsT=x32[:, dc, s2 * 128:(s2 + 1) * 128], rhs=rwt[:, dc, :],
                                                                            start=(dc == 0), stop=(dc == 7)),
                                     [bx32, B_rwt], [bpr], pe_accum=True)
                        S.op("act", lambda e, sub=sub: e.activation(out=sc_all[:, 2 * sub:2 * sub + 2, :], in_=pr[:, 0:32].rearrange("p (a b) -> p a b", a=2), func=AF.Sigmoid),
                             [bpr], [B_sc])
                    sel, bsel = rt["sel"]
                    S.op("dve", lambda e: e.tensor_tensor(out=sel[:], in0=sc_all[:], in1=rtb[:].unsqueeze(1).broadcast_to([128, 8, 16]), op=ALU.add),
                         [B_sc, B_rtb], [bsel])
                    sel4 = sel[:].rearrange("p s (g j) -> p (s g) j", j=4)
                    m1, bm1 = small["m1"]
                    m1v = m1[:].rearrange("p s g -> p (s g)")
                    S.op("dve", lambda e: e.tensor_reduce(out=m1v, in_=sel4, axis=AX.X, op=ALU.max), [bsel], [bm1])
                    eq, beq = rt["eq"]
                    eq4 = eq[:].rearrange("p s (g j) -> p (s g) j", j=4)
                    S.op("dve", lambda e: e.tensor_tensor(out=eq4, in0=sel4, in1=m1v.unsqueeze(2).broadcast_to([128, 32, 4]), op=ALU.is_ge), [bsel, bm1], [beq])
                    S.op("dve", lambda e: e.scalar_tensor_tensor(out=eq4, in0=eq4, scalar=-1e9, in1=sel4, op0=ALU.mult, op1=ALU.add), [beq, bsel], [beq])
                    m2, bm2 = small["m2"]
                    m2v = m2[:].rearrange("p s g -> p (s g)")
                    S.op("dve", lambda e: e.tensor_reduce(out=m2v, in_=eq4, axis=AX.X, op=ALU.max), [beq], [bm2])
                    gs, bgs = small["gs"]
                    S.op("dve", lambda e: e.tensor_tensor(out=gs[:], in0=m1[:], in1=m2[:], op=ALU.add), [bm1, bm2], [bgs])
                    gmax, bgmax = small1["gmax"]
                    S.op("dve", lambda e: e.tensor_reduce(out=gmax[:], in_=gs[:], axis=AX.X, op=ALU.max), [bgs], [bgmax])
                    gsel, bgsel = small["gsel"]
                    S.op("dve", lambda e: e.tensor_tensor(out=gsel[:], in0=gs[:], in1=gmax[:].unsqueeze(2).broadcast_to([128, 8, 4]), op=ALU.is_ge), [bgs, bgmax], [bgsel])
                    msk, bmsk = rt["msk"]
                    msk4 = msk[:].rearrange("p s (g j) -> p (s g) j", j=4)
                    S.op("dve", lambda e: e.tensor_tensor(out=msk4, in0=sel4, in1=m2v.unsqueeze(2).broadcast_to([128, 32, 4]), op=ALU.is_ge), [bsel, bm2], [bmsk])
                    S.op("dve", lambda e: e.tensor_tensor(out=msk4, in0=msk4, in1=gsel[:].rearrange("p s g -> p (s g)").unsqueeze(2).broadcast_to([128, 32, 4]), op=ALU.mult),
                         [bmsk, bgsel], [bmsk])
                    gate, bgate = rt["gate"]
                    S.op("dve", lambda e: e.tensor_tensor(out=gate[:], in0=msk[:], in1=sc_all[:], op=ALU.mult), [bmsk, B_sc], [bgate])
                    den, bden = small1["den"]
                    S.op("dve", lambda e: e.tensor_reduce(out=den[:], in_=gate[:], axis=AX.X, op=ALU.add), [bgate], [bden])
                    S.op("dve", lambda e: e.reciprocal(out=den[:], in_=den[:]), [bden], [bden])
                    comb, bcomb = rt["comb"]
                    S.op("dve", lambda e: e.tensor_tensor(out=comb[:], in0=gate[:], in1=den[:].unsqueeze(2).broadcast_to([128, 8, 16]), op=ALU.mult), [bgate, bden], [bcomb])
                    for s8 in range(8):
                        pc, bpc = pms.next()
                        S.op("pe", lambda e, s8=s8: e.transpose(pc[0:16, 0:128], comb[:, s8, :], identf), [bcomb, B_cst], [bpc])
                        copy("act", combT[:, s8 * 128:(s8 + 1) * 128], pc[0:16, 0:128], [bpc], [B_combT])
                    if dbg and "d_comb" in dbg_d and l == 0 and tt == 0:
                        S.dma("sp", dbg_d["d_comb"], comb[:].rearrange("p a b -> p (a b)"), reads=[bcomb], writes=[B_dbg["d_comb"]])
                    while pending:
                        resid_prev(pending.pop(0))
                    castn = {"i": 0}

                    def load_expert(x_):
                        wg_, bwg = wgu.next()
                        wd_, bwd = wdn.next()
                        pieces = []
                        for half in range(2):
                            pieces.append((wg_[:, 0, half * 4:(half + 1) * 4, :], bwg, mg_d[l, x_, half * 512:(half + 1) * 512, :].rearrange("(c p) e -> p c e", p=128), [128, 4, 512]))
                        for half in range(2):
                            pieces.append((wg_[:, 1, half * 4:(half + 1) * 4, :], bwg, mu_d[l, x_, half * 512:(half + 1) * 512, :].rearrange("(c p) e -> p c e", p=128), [128, 4, 512]))
                        for half in range(2):
                            pieces.append((wd_[:, half * 2:(half + 1) * 2, :], bwd, md_d[l, x_, half * 256:(half + 1) * 256, :].rearrange("(c p) e -> p c e", p=128), [128, 2, 1024]))
                        for dst, bdst, src, shape in pieces:
                            st, bst = stg.next()
                            stv = st[:, 0:shape[1] * shape[2]].rearrange("p (a b) -> p a b", a=shape[1])
                            S.dma("sp", stv, src, writes=[bst])
                            castn["i"] += 1
                            if castn["i"] % 2 == 0:
                                S.op("pool", lambda e, dst=dst, stv=stv: e.tensor_copy(out=dst, in_=stv), [bst], [bdst])
                            else:
                                S.op("act", lambda e, dst=dst, stv=stv: e.activation(out=dst, in_=stv, func=AF.Copy), [bst], [bdst])
                        return wg_, bwg, wd_, bwd

                    def expert_sub(x_, sub, wg_, bwg, wd_, bwd):
                        ssl = slice(sub * NX, (sub + 1) * NX)
                        pc, bpc = pgu.next()
                        S.op("pe", lambda e: e.matmul(pc[:], lhsT=selT[x_], rhs=combT[:, ssl], start=True, stop=True),
                             [B_combT, B_sel], [bpc])
                        cb, bcb = cbs.next()
                        copy("act", cb[:], pc[:], [bpc], [bcb])
                        he, bhe = hes.next()
                        for f in range(4):
                            pg, bpg = pgu.next()
                            pu, bpu = pgu.next()
                            for dc in range(8):
                                S.op("pe", lambda e, dc=dc, f=f: e.matmul(pg[:], lhsT=wg_[:, 0, dc, f * 128:(f + 1) * 128], rhs=xm[:, dc, ssl], start=(dc == 0), stop=(dc == 7)),
                                     [bwg, B_xm], [bpg], pe_accum=True)
                            for dc in range(8):
                                S.op("pe", lambda e, dc=dc, f=f: e.matmul(pu[:], lhsT=wg_[:, 1, dc, f * 128:(f + 1) * 128], rhs=xm[:, dc, ssl], start=(dc == 0), stop=(dc == 7)),
                                     [bwg, B_xm], [bpu], pe_accum=True)
                            sg, bsg = sgs.next()
                            S.op("act", lambda e: e.activation(out=sg[:], in_=pg[:], func=AF.Silu), [bpg], [bsg])
                            t1, bt1 = t1s.next()
                            S.op("dve", lambda e: e.tensor_tensor(out=t1[:], in0=pu[:], in1=sg[:], op=ALU.mult), [bpu, bsg], [bt1])
                            S.op("dve", lambda e, f=f: e.tensor_tensor(out=he[:, f, :], in0=t1[:], in1=cb[:], op=ALU.mult), [bt1, bcb], [bhe])
                        yield
                        for eo in range(8):
                            pd, bpd = pdn.next()
                            for f in range(4):
                                S.op("pe", lambda e, f=f, eo=eo: e.matmul(pd[:], lhsT=wd_[:, f, eo * 128:(eo + 1) * 128], rhs=he[:, f, :], start=(f == 0), stop=(f == 3)),
                                     [bwd, bhe], [bpd], pe_accum=True)
                            if x_ == 0:
                                copy("dve" if eo % 2 == 0 else "act", acc[:, eo, ssl], pd[:], [bpd], [B_acc[sub]])
                            else:
                                S.op("dve", lambda e, eo=eo: e.tensor_tensor(out=acc[:, eo, ssl], in0=pd[:], in1=acc[:, eo, ssl], op=ALU.add), [bpd, B_acc[sub]], [B_acc[sub]])
                        yield

                    _noload = False
                    nxt = load_expert(0)
                    for x_ in range(NE):
                        curw = nxt
                        if x_ + 1 < NE and not _noload:
                            nxt = load_expert(x_ + 1)
                        run_lockstep([expert_sub(x_, sub, *curw) for sub in range(NSX)])
                    def resid(tt_):
                        for sub in range(NS):
                            ssl = slice(sub * NT, (sub + 1) * NT)
                            tsl = slice(tt_ * TT + sub * NT, tt_ * TT + (sub + 1) * NT)
                            ht, bht = hin.next()
                            S.dma(dmaq.next(), ht[:], hT_d[:, :, tsl], reads=[B_hT], writes=[bht])
                            for fc in range(8):
                                S.op("dve", lambda e, fc=fc: e.scalar_tensor_tensor(out=ht[:, fc, :], in0=acc[:, fc, ssl], scalar=gt_ap(l, 1, fc), in1=ht[:, fc, :],
                                                                                                            op0=ALU.mult, op1=ALU.add), [B_acc[(sub * NT) // NX], bht, B_mod], [bht])
                            S.dma(dmaq.next(), hT_d[:, :, tsl], ht[:], reads=[bht], writes=[B_hT])
                    resid_prev = resid
                    pending.append(tt)
                while pending:
                    resid(pending.pop(0))
                S.barrier()

        selall = sbt(es, "selall", [16, NE, 128], BF16)
        B_sel = Buf()
        S.op("pool", lambda e: e.memset(selall[:], 0.0), [], [B_sel])
        for x_ in range(NE):
            S.op("pool", lambda e, x_=x_: e.tensor_copy(out=selall[:, x_, :], in_=cst[0:16, x_:x_ + 1].broadcast_to([16, 128])), [B_cst], [B_sel])
        selT = [selall[:, x_, :] for x_ in range(NE)]

        moe_phase(0)
        if stop_after == "moe0":
            return finish([B_hT])

        with contextlib.ExitStack() as ph:
            win = sbt(ph, "win", [128, 8, 3072], BF16)
            wout = sbt(ph, "wout", [128, 8, 1024], BF16)
            B_w1 = Buf()
            with contextlib.ExitStack() as ph2:
                stg = Ring([(sbt(ph2, "stgc%d" % i, [128, 8192], F32), Buf()) for i in range(2)])
                for p_ in range(3):
                    load_w(stg, win[:, :, p_ * 1024:(p_ + 1) * 1024], B_w1, win_d[:, p_ * 1024:(p_ + 1) * 1024].rearrange("(c p) e -> p c e", p=128), [128, 8, 1024], "win")
                load_w(stg, wout[:], B_w1, wout_d.rearrange("(c p) e -> p c e", p=128), [128, 8, 1024], "wout")
                S.barrier()
            hr = ring_tiles(ph, "hc", [128, 8, NT], F32, 2)
            xnr_ = ring_tiles(ph, "xc", [128, 8, NT], F32, 2)
            xbr = ring_tiles(ph, "xcb", [128, 8, NT], BF16, 2)
            zr = ring_tiles(ph, "zc", [128, 8, NT], BF16, 2)
            sqt = sbt(ph, "sqc", [128, 8, NT], BF16)
            B_sq = Buf()
            rstd = sbt(ph, "rstdc", [128, NT], F32)
            B_rstd = Buf()
            wkr = ring_tiles(ph, "wkc", [128, NT], F32, 8)
            psr = pslots2(ph, "psc", 14)
            def cv_tile(ti):
                tsl = slice(ti * NT, (ti + 1) * NT)
                ht, bht = hr.next()
                S.dma(dmaq.next(), ht[:], hT_d[:, :, tsl], reads=[B_hT], writes=[bht])
                xn_, bxn = xnr_.next()
                pn, bpn = psr.next()
                norm_mod(ht, bht, NT, xn_, bxn, lambda fc: sc_ap(1, 0, fc), lambda fc: sh_ap(1, 0, fc, 0), sqt, B_sq, rstd, B_rstd, pn, bpn)
                yield
                xb, bxb = xbr.next()
                copy("pool", xb[:], xn_[:], [bxn], [bxb])
                z, bz = zr.next()
                for fc in range(8):
                    pp = []
                    for p_ in range(3):
                        ps_, bps = psr.next()
                        for dc in range(8):
                            S.op("pe", lambda e, dc=dc, p_=p_: e.matmul(ps_[:], lhsT=win[:, dc, p_ * 1024 + fc * 128: p_ * 1024 + (fc + 1) * 128], rhs=xb[:, dc, :],
                                                                       start=(dc == 0), stop=(dc == 7)), [bxb, B_w1], [bps], pe_accum=True)
                        pp.append((ps_, bps))
                    (pbg, bpbg), (pcg, bpcg), (pxi, bpxi) = pp
                    yield
                    xi, bxi = wkr.next()
                    copy("act", xi[:], pxi[:], [bpxi], [bxi])
                    u, bu = wkr.next()
                    S.op("dve", lambda e: e.tensor_tensor(out=u[:], in0=pcg[:], in1=xi[:], op=ALU.mult), [bpcg, bxi], [bu])
                    cv_, bcv = wkr.next()
                    S.op("act", lambda e: e.activation(out=cv_[:], in_=u[:], func=AF.Copy, scale=vecs[:, 20, fc:fc + 1]), [bu, B_vecs], [bcv])
                    u3 = u[:].rearrange("p (r t) -> p r t", t=64)
                    c3 = cv_[:].rearrange("p (r t) -> p r t", t=64)
                    S.op("dve", lambda e: e.scalar_tensor_tensor(out=c3[:, :, 1:64], in0=u3[:, :, 0:63], scalar=vecs[:, 19, fc:fc + 1], in1=c3[:, :, 1:64], op0=ALU.mult, op1=ALU.add),
                         [bu, bcv, B_vecs], [bcv])
                    S.op("dve", lambda e: e.scalar_tensor_tensor(out=c3[:, :, 0:63], in0=u3[:, :, 1:64], scalar=vecs[:, 21, fc:fc + 1], in1=c3[:, :, 0:63], op0=ALU.mult, op1=ALU.add),
                         [bu, bcv, B_vecs], [bcv])
                    S.op("dve", lambda e: e.tensor_tensor(out=z[:, fc, :], in0=pbg[:], in1=cv_[:], op=ALU.mult), [bpbg, bcv], [bz])
                for fo in range(8):
                    yield
                    po, bpo = psr.next()
                    for dc in range(8):
                        S.op("pe", lambda e, dc=dc: e.matmul(po[:], lhsT=wout[:, dc, fo * 128:(fo + 1) * 128], rhs=z[:, dc, :], start=(dc == 0), stop=(dc == 7)),
                             [bz, B_w1], [bpo], pe_accum=True)
                    S.op("dve", lambda e: e.scalar_tensor_tensor(out=ht[:, fo, :], in0=po[:], scalar=gt_ap(1, 0, fo), in1=ht[:, fo, :], op0=ALU.mult, op1=ALU.add),
                         [bpo, bht, B_mod], [bht])
                S.dma(dmaq.next(), hT_d[:, :, tsl], ht[:], reads=[bht], writes=[B_hT])
            for t2 in range(0, T // NT, 2):
                run_lockstep([cv_tile(t2), cv_tile(t2 + 1)])
            S.barrier()

        moe_phase(1)

        with contextlib.ExitStack() as ph:
            hr = ring_tiles(ph, "hf", [128, 8, NT], F32, 2)
            xnr_ = ring_tiles(ph, "xf", [128, 8, NT], F32, 2)
            otr = ring_tiles(ph, "of", [128, 1024], F32, 2)
            sqt = sbt(ph, "sqf", [128, 8, NT], BF16)
            B_sq = Buf()
            rstd = sbt(ph, "rstdf", [128, NT], F32)
            B_rstd = Buf()
            pnr = pslots2(ph, "pnf", 2)
            pxr = Ring([(pst(ph, "pxf%d" % i, [128, 4, 128], F32), PBuf()) for i in range(4)])
            def fin_tile(ti):
                tsl = slice(ti * NT, (ti + 1) * NT)
                ht, bht = hr.next()
                S.dma(dmaq.next(), ht[:], hT_d[:, :, tsl], reads=[B_hT], writes=[bht])
                xn_, bxn = xnr_.next()
                pn, bpn = pnr.next()
                norm_mod(ht, bht, NT, xn_, bxn, lambda fc: vecs[:, 22, fc:fc + 1], None, sqt, B_sq, rstd, B_rstd, pn, bpn)
                for sub in range(2):
                    yield
                    ot, bot = otr.next()
                    for half in range(2):
                        px, bpx = pxr.next()
                        for f4 in range(4):
                            fc = half * 4 + f4
                            S.op("pe", lambda e, fc=fc, f4=f4: e.transpose(px[:, f4, :], xn_[:, fc, sub * 128:(sub + 1) * 128], identf), [bxn, B_cst], [bpx], pe_accum=True)
                        copy("dve" if half == 0 else "act", ot[:, half * 512:(half + 1) * 512], px[:].rearrange("p a b -> p (a b)"), [bpx], [bot])
                    r0 = ti * NT + sub * 128
                    S.dma(dmaq.next(), out_d[r0:r0 + 128, :], ot[:], reads=[bot], writes=[B_out])
            for t2 in range(0, T // NT, 2):
                run_lockstep([fin_tile(t2), fin_tile(t2 + 1)])
            S.final_wait("sp", [B_out] + list(B_dbg.values()))
            S.final_wait("pool", [B_out])
    return nc


def _consts():
    c = np.zeros((128, 1152), np.float32)
    i = np.arange(128)
    c[:, 0:128] = np.eye(128)
    c[:, 128:256] = (i[:, None] < i[None, :])
    c[:, 256:384] = (i[:, None] <= i[None, :])
    c[:, 384:512] = (i[:, None] > i[None, :])
    c[:, 512:640] = (i[:, None] >= i[None, :])
    c[:, 640:768] = ((i[:, None] // 64) == (i[None, :] // 64))
    c[:, 768:896] = 1.0
    m = np.ones(256, np.float32)
    m[0::128] = 0.0
    c[:, 896:1152] = m[None, :]
    return c


def _pvec(v):
    return np.ascontiguousarray(np.asarray(v, np.float32).reshape(8, 128).T)


def prep_inputs(inp):
    f = lambda a: np.ascontiguousarray(np.asarray(a, np.float32))
    vec_list = [inp["norm_g"][0, 0], inp["norm_g"][0, 1], inp["norm_g"][1, 0], inp["norm_g"][1, 1]]
    vec_list += [inp["rw_mu"][0, i] for i in range(6)]
    vec_list += [inp["rw_w0"][0, 0], inp["rw_w0"][0, 1], inp["rw_a0"][0, 0], inp["rw_a0"][0, 1]]
    vec_list += [inp["rw_k_k"][0], inp["rw_k_a"][0], np.asarray(inp["rw_r_k"][0]).reshape(-1), inp["rw_gn_w"][0], inp["rw_gn_b"][0]]
    vec_list += [inp["sc_conv"][0, i] for i in range(3)]
    vec_list += [inp["final_g"]]
    assert len(vec_list) == NV
    vecs = np.stack([_pvec(v) for v in vec_list], axis=1).reshape(128, NV * 8)
    adab = np.asarray(inp["ada_b"], np.float32).reshape(2, 48, 128).transpose(2, 0, 1).reshape(128, 96)
    shared = {
        "ada_w": f(inp["ada_w"]), "adab": f(adab), "vecs": f(vecs), "cst": _consts(),
        "rtb": f(np.broadcast_to(np.asarray(inp["router_b"], np.float32)[None, :], (128, 16))),
        "w_rkv": f(inp["rw_w_rkv"][0]), "w1": f(inp["rw_w1"][0]), "w2": f(inp["rw_w2"][0]),
        "a1": f(inp["rw_a1"][0]), "a2": f(inp["rw_a2"][0]), "g1": f(inp["rw_g1"][0]), "g2": f(inp["rw_g2"][0]),
        "w_o": f(inp["rw_w_o"][0]), "w_in": f(inp["sc_w_in"][0]), "w_out": f(inp["sc_w_out"][0]),
        "router_w": f(inp["router_w"]), "moe_g": f(inp["moe_w_gate"]), "moe_u": f(inp["moe_w_up"]), "moe_d": f(inp["moe_w_down"]),
    }
    maps = []
    cc = _pvec(inp["c_ctx"])
    for b in range(NCORE):
        m = dict(shared)
        m["x"] = f(inp["x"][b])
        m["ctx"] = f(inp["ctx"][b])
        m["cvec"] = f(np.stack([_pvec(inp["c"][b]), cc], axis=2).reshape(128, 16))
        maps.append(m)
    return maps


_NC_CACHE = {}


def kernel(**inputs):
    maps = prep_inputs(inputs)
    if "nc" not in _NC_CACHE:
        _NC_CACHE["nc"] = build_program()
    res = run_bass_kernel_spmd(_NC_CACHE["nc"], maps, core_ids=list(range(NCORE)))
    return np.stack([np.asarray(r["out"], np.float32) for r in res.results], axis=0)
```
